# Optimizing a Trainium2 kernel written in Bass

```python
import math
import jax, jax.numpy as jnp
from jax import lax
import numpy as np

D_MODEL = 2048
BATCH = 8
SEQ = 4096
DEPTH = 4

CHUNK = 64
Q_BLOCK = 128
N_BRANCH = 4
BRANCH_WIDTH = D_MODEL // 4
DIFF_HEADS = 4
DIFF_QK_DIM = BRANCH_WIDTH // (2 * DIFF_HEADS)
DIFF_V_DIM = BRANCH_WIDTH // DIFF_HEADS
GLA_HEADS = 4
GLA_DK = BRANCH_WIDTH // (2 * GLA_HEADS)
GLA_DV = BRANCH_WIDTH // GLA_HEADS
GLA_GATE_RANK = 16
GLA_GATE_NORM = 16.0
MLSTM_HEADS = 4
MLSTM_DH = BRANCH_WIDTH // MLSTM_HEADS
MLSTM_CONV = 4
CA_HEADS = 4
CA_DH = BRANCH_WIDTH // CA_HEADS
CA_LEFT_CHUNKS = 8
REL_CLIP = 2 * CHUNK
D_FF = 4 * D_MODEL
EPS = 1e-6
DIFF_COLS = 3 * BRANCH_WIDTH
GLA_COLS = 2 * GLA_HEADS * GLA_DK + BRANCH_WIDTH + GLA_GATE_RANK + BRANCH_WIDTH
MLSTM_COLS = 4 * BRANCH_WIDTH + 2 * MLSTM_HEADS
CA_COLS = 3 * BRANCH_WIDTH
N_IN = DIFF_COLS + GLA_COLS + MLSTM_COLS + CA_COLS

kernel_name = 'hybrid_gated_streaming_encoder'


def rms_norm(x, g):
    xf = x.astype(jnp.float32)
    y = xf * lax.rsqrt(jnp.mean(xf * xf, axis=-1, keepdims=True) + EPS)
    return (y * g.astype(jnp.float32)).astype(x.dtype)


def head_rms_norm(x, g):
    H, d = x.shape[-2], x.shape[-1]
    return rms_norm(x, g.reshape(H, d))


def alibi_slopes(n):
    return jnp.asarray([2.0 ** (-8.0 * (h + 1) / n) for h in range(n)], jnp.float32)


def causal_conv(x, w, b):
    K, C = w.shape
    y = lax.conv_general_dilated(x, w[:, None, :], window_strides=(1,), padding=[(K - 1, 0)],
                                 dimension_numbers=('NWC', 'WIO', 'NWC'), feature_group_count=C)
    return y + b


def diff_attention(u, lam, gain, layer_idx):
    B, T, _ = u.shape
    q, k, v = jnp.split(u, 3, axis=-1)
    q = q.reshape(B, T, DIFF_HEADS, 2, DIFF_QK_DIM)
    k = k.reshape(B, T, DIFF_HEADS, 2, DIFF_QK_DIM)
    v = v.reshape(B, T, DIFF_HEADS, DIFF_V_DIM)
    lam_init = 0.8 - 0.6 * math.exp(-0.3 * layer_idx)
    lf = lam.astype(jnp.float32)
    lam_full = jnp.exp(jnp.sum(lf[0] * lf[1])) - jnp.exp(jnp.sum(lf[2] * lf[3])) + lam_init
    slopes = alibi_slopes(DIFF_HEADS)[None, :, None, None, None]
    scale = DIFF_QK_DIM ** -0.5
    pos = jnp.arange(T)
    outs = []
    for blk in range(T // Q_BLOCK):
        q0, q1 = blk * Q_BLOCK, (blk + 1) * Q_BLOCK
        tq, tk = pos[q0:q1], pos[:q1]
        s = jnp.einsum('bqhmd,bkhmd->bhmqk', q[:, q0:q1], k[:, :q1]).astype(jnp.float32) * scale
        dist = jnp.abs(tq[:, None] - tk[None, :]).astype(jnp.float32)
        allowed = (tk[None, :] // CHUNK) <= (tq[:, None] // CHUNK)
        s = jnp.where(allowed, s - slopes * dist, -jnp.inf)
        p = jax.nn.softmax(s, axis=-1)
        a = p[:, :, 0] - lam_full * p[:, :, 1]
        outs.append(jnp.einsum('bhqk,bkhd->bqhd', a.astype(v.dtype), v[:, :q1]))
    o = jnp.concatenate(outs, axis=1)
    o = head_rms_norm(o, gain) * (1.0 - lam_init)
    return o.reshape(B, T, -1)


def gla(u, w_gate_up, b_gate, gain):
    B, T, _ = u.shape
    H, dk, dv, L = GLA_HEADS, GLA_DK, GLA_DV, CHUNK
    nc = T // L
    c1 = H * dk
    q, k, v, g_low, r = jnp.split(u, [c1, 2 * c1, 2 * c1 + BRANCH_WIDTH, 2 * c1 + BRANCH_WIDTH + GLA_GATE_RANK], axis=-1)
    log_a = jax.nn.log_sigmoid((g_low @ w_gate_up + b_gate).astype(jnp.float32)) / GLA_GATE_NORM
    log_a = log_a.reshape(B, nc, L, H, dk)
    q = q.reshape(B, nc, L, H, dk).astype(jnp.float32) * dk ** -0.5
    k = k.reshape(B, nc, L, H, dk).astype(jnp.float32)
    v = v.reshape(B, nc, L, H, dv).astype(jnp.float32)
    cum = jnp.cumsum(log_a, axis=2)
    total = cum[:, :, -1]
    q_dec = q * jnp.exp(cum)
    k_inv = k * jnp.exp(-cum)
    k_tail = k * jnp.exp(total[:, :, None] - cum)
    causal = jnp.tril(jnp.ones((L, L), dtype=bool))
    att = jnp.where(causal, jnp.einsum('bcthk,bcshk->bchts', q_dec, k_inv), 0.0)
    o_intra = jnp.einsum('bchts,bcshv->bcthv', att, v)
    dS = jnp.einsum('bcshk,bcshv->bchkv', k_tail, v)

    def step(S, inp):
        decay, inc = inp
        return jnp.exp(decay)[..., None] * S + inc, S

    S0 = jnp.zeros((B, H, dk, dv), jnp.float32)
    _, S_prev = lax.scan(step, S0, (jnp.moveaxis(total, 1, 0), jnp.moveaxis(dS, 1, 0)))
    S_prev = jnp.moveaxis(S_prev, 0, 1)
    o_inter = jnp.einsum('bcthk,bchkv->bcthv', q_dec, S_prev)
    o = (o_intra + o_inter).reshape(B, T, H, dv)
    o = head_rms_norm(o, gain).reshape(B, T, -1) * jax.nn.silu(r.astype(jnp.float32))
    return o.astype(u.dtype)


def mlstm(u, conv_w, conv_b, b_i, b_f, gain):
    B, T, _ = u.shape
    H, d, L, W = MLSTM_HEADS, MLSTM_DH, CHUNK, BRANCH_WIDTH
    nc = T // L
    qk, v, o_pre, i_pre, f_pre = jnp.split(u, [2 * W, 3 * W, 4 * W, 4 * W + H], axis=-1)
    qk = jax.nn.silu(causal_conv(qk, conv_w, conv_b))
    q, k = jnp.split(qk, 2, axis=-1)
    hm = lambda a: a.reshape(B, nc, L, H, d).astype(jnp.float32).transpose(0, 1, 3, 2, 4)
    q, k, v = hm(q), hm(k) * d ** -0.5, hm(v)
    log_i = jnp.swapaxes((i_pre.astype(jnp.float32) + b_i).reshape(B, nc, L, H), 2, 3)
    log_f = jnp.swapaxes(jax.nn.log_sigmoid(f_pre.astype(jnp.float32) + b_f).reshape(B, nc, L, H), 2, 3)
    bcum = jnp.cumsum(log_f, axis=-1)
    btot = bcum[..., -1]
    causal = jnp.tril(jnp.ones((L, L), dtype=bool))
    Dlog = jnp.where(causal, bcum[..., :, None] - bcum[..., None, :] + log_i[..., None, :], -jnp.inf)
    tail = btot[..., None] - bcum + log_i
    m_loc = jnp.max(tail, axis=-1)
    w_tail = jnp.exp(tail - m_loc[..., None])
    dC = jnp.einsum('bchs,bchsk,bchsv->bchkv', w_tail, k, v)
    dn = jnp.einsum('bchs,bchsk->bchk', w_tail, k)

    def step(carry, inp):
        C, n, m = carry
        g, ml, dCc, dnc = inp
        m_new = jnp.maximum(g + m, ml)
        a, bb = jnp.exp(g + m - m_new), jnp.exp(ml - m_new)
        C_new = a[..., None, None] * C + bb[..., None, None] * dCc
        n_new = a[..., None] * n + bb[..., None] * dnc
        return (C_new, n_new, m_new), (C, n, m)

    init = (jnp.zeros((B, H, d, d), jnp.float32), jnp.zeros((B, H, d), jnp.float32), jnp.zeros((B, H), jnp.float32))
    mv = lambda a: jnp.moveaxis(a, 1, 0)
    _, (C_prev, n_prev, m_prev) = lax.scan(step, init, (mv(btot), mv(m_loc), mv(dC), mv(dn)))
    C_prev, n_prev, m_prev = jnp.moveaxis(C_prev, 0, 1), jnp.moveaxis(n_prev, 0, 1), jnp.moveaxis(m_prev, 0, 1)
    inter_log = bcum + m_prev[..., None]
    m_t = jnp.maximum(jnp.max(Dlog, axis=-1), inter_log)
    A = jnp.exp(Dlog - m_t[..., None]) * jnp.einsum('bchtk,bchsk->bchts', q, k)
    inter_w = jnp.exp(inter_log - m_t)
    num = jnp.einsum('bchts,bchsv->bchtv', A, v) + inter_w[..., None] * jnp.einsum('bchtk,bchkv->bchtv', q, C_prev)
    den = jnp.sum(A, axis=-1) + inter_w * jnp.einsum('bchtk,bchk->bcht', q, n_prev)
    h = num / jnp.maximum(jnp.abs(den), jnp.exp(-m_t))[..., None]
    h = h.transpose(0, 1, 3, 2, 4).reshape(B, T, H, d)
    o_gate = jax.nn.sigmoid(o_pre.astype(jnp.float32)).reshape(B, T, H, d)
    return head_rms_norm(o_gate * h, gain).reshape(B, T, -1).astype(u.dtype)


def chunk_band_attention(u, rel_table):
    B, T, _ = u.shape
    H, d, L = CA_HEADS, CA_DH, CHUNK
    nc, nb = T // L, CA_LEFT_CHUNKS + 1
    q, k, v = jnp.split(u, 3, axis=-1)
    q = q.reshape(B, nc, L, H, d)
    pad = ((0, 0), (CA_LEFT_CHUNKS, 0), (0, 0), (0, 0), (0, 0))
    kp = jnp.pad(k.reshape(B, nc, L, H, d), pad)
    vp = jnp.pad(v.reshape(B, nc, L, H, d), pad)
    band_idx = jnp.arange(nc)[:, None] + jnp.arange(nb)[None, :]
    kb = kp[:, band_idx].reshape(B, nc, nb * L, H, d)
    vb = vp[:, band_idx].reshape(B, nc, nb * L, H, d)
    s = jnp.einsum('bcqhd,bckhd->bchqk', q, kb).astype(jnp.float32) * d ** -0.5
    i = jnp.arange(L)
    j = jnp.arange(nb)
    key_off = ((nb - 1 - j)[:, None] * L - i[None, :]).reshape(-1)
    rel = i[:, None] + key_off[None, :]
    bias = rel_table[:, jnp.clip(rel, -REL_CLIP, REL_CLIP) + REL_CLIP].astype(jnp.float32)
    valid = jnp.repeat(band_idx >= CA_LEFT_CHUNKS, L, axis=1)
    s = jnp.where(valid[None, :, None, None, :], s + bias[None, None], -jnp.inf)
    p = jax.nn.softmax(s, axis=-1)
    o = jnp.einsum('bchqk,bckhd->bcqhd', p.astype(vb.dtype), vb)
    return o.reshape(B, T, H * d)


def setup_inputs(seed: int = 0) -> dict:
    key = jax.random.key(seed)
    ks = jax.random.split(key, 24)
    f32 = jnp.float32
    nrm = lambda k, shape, scale: scale * jax.random.normal(k, shape, f32)
    gain = lambda k, shape: 1.0 + 0.05 * jax.random.normal(k, shape, f32)
    L, D, W = DEPTH, D_MODEL, BRANCH_WIDTH
    return {
        'x': nrm(ks[0], (BATCH, SEQ, D), 1.0),
        'norm_mix_pre': gain(ks[1], (L, D)),
        'norm_mix_post': gain(ks[2], (L, D)),
        'w_in': nrm(ks[3], (L, D, N_IN), D ** -0.5),
        'diff_lambda': nrm(ks[4], (L, 4, DIFF_QK_DIM), 0.1),
        'diff_norm': gain(ks[5], (L, W)),
        'gla_w_gate_up': nrm(ks[6], (L, GLA_GATE_RANK, GLA_HEADS * GLA_DK), GLA_GATE_RANK ** -0.5),
        'gla_b_gate': nrm(ks[7], (L, GLA_HEADS * GLA_DK), 0.1),
        'gla_norm': gain(ks[8], (L, W)),
        'mlstm_conv_w': nrm(ks[9], (L, MLSTM_CONV, 2 * W), MLSTM_CONV ** -0.5),
        'mlstm_conv_b': nrm(ks[10], (L, 2 * W), 0.01),
        'mlstm_b_i': nrm(ks[11], (L, MLSTM_HEADS), 0.1),
        'mlstm_b_f': jnp.linspace(3.0, 6.0, MLSTM_HEADS, dtype=f32)[None, :] + nrm(ks[12], (L, MLSTM_HEADS), 0.1),
        'mlstm_norm': gain(ks[13], (L, W)),
        'rel_bias': nrm(ks[14], (L, CA_HEADS, 2 * REL_CLIP + 1), 0.1),
        'w_branch': nrm(ks[15], (L, N_BRANCH, W, D), W ** -0.5),
        'w_gate': nrm(ks[16], (L, N_BRANCH, D, D), D ** -0.5),
        'b_gate': nrm(ks[17], (L, N_BRANCH, D), 0.01),
        'w_out': nrm(ks[18], (L, D, D), D ** -0.5),
        'norm_ffn_pre': gain(ks[19], (L, D)),
        'norm_ffn_post': gain(ks[20], (L, D)),
        'w_up': nrm(ks[21], (L, D, D_FF), D ** -0.5),
        'w_down': nrm(ks[22], (L, D_FF, D), D_FF ** -0.5),
    }


def reference(x, norm_mix_pre, norm_mix_post, w_in, diff_lambda, diff_norm, gla_w_gate_up, gla_b_gate,
              gla_norm, mlstm_conv_w, mlstm_conv_b, mlstm_b_i, mlstm_b_f, mlstm_norm, rel_bias, w_branch,
              w_gate, b_gate, w_out, norm_ffn_pre, norm_ffn_post, w_up, w_down):
    for l in range(DEPTH):
        h = rms_norm(x, norm_mix_pre[l])
        u = h @ w_in[l]
        u_a, u_b, u_c, u_d = jnp.split(u, [DIFF_COLS, DIFF_COLS + GLA_COLS, DIFF_COLS + GLA_COLS + MLSTM_COLS], axis=-1)
        branches = [
            diff_attention(u_a, diff_lambda[l], diff_norm[l], l),
            gla(u_b, gla_w_gate_up[l], gla_b_gate[l], gla_norm[l]),
            mlstm(u_c, mlstm_conv_w[l], mlstm_conv_b[l], mlstm_b_i[l], mlstm_b_f[l], mlstm_norm[l]),
            chunk_band_attention(u_d, rel_bias[l]),
        ]
        merged = None
        for bi in range(N_BRANCH):
            gate = jax.nn.sigmoid(h @ w_gate[l, bi] + b_gate[l, bi])
            term = gate * (branches[bi] @ w_branch[l, bi])
            merged = term if merged is None else merged + term
        x = x + rms_norm(merged @ w_out[l], norm_mix_post[l])
        h2 = rms_norm(x, norm_ffn_pre[l])
        f = jnp.square(jax.nn.relu(h2 @ w_up[l])) @ w_down[l]
        x = x + rms_norm(f, norm_ffn_post[l])
    return x
```

```python
import math
import numpy as np
import ml_dtypes
from contextlib import ExitStack
import concourse.bass as bass
import concourse.mybir as mybir
from concourse.bass_utils import run_bass_kernel_spmd

F32 = mybir.dt.float32
BF16 = mybir.dt.bfloat16
AF = mybir.ActivationFunctionType
ALU = mybir.AluOpType
AX = mybir.AxisListType

D = 2048
DC = 16
DFF = 8192
NIN = 6680
EPS = 1e-6
A0, B0, C0, D0 = 0, 1536, 3088, 5144
ENGS = ("pe", "act", "dve", "pool", "sp")


class Sched:
    def __init__(self, nc, es, n_dma_sems=10):
        self.nc = nc
        self.sem = {e: es.enter_context(nc.semaphore("s_" + e)) for e in ENGS}
        self.cnt = {e: 0 for e in ENGS}
        self.dq = ("sp", "pool")
        self.dsems = {q: [es.enter_context(nc.semaphore(f"d_{q}{i}")) for i in range(n_dma_sems)] for q in self.dq}
        self.dcnt = {q: [0] * n_dma_sems for q in self.dq}
        self.dnext = {q: 0 for q in self.dq}
        self.seen = {e: {} for e in ENGS}
        self.bar = es.enter_context(nc.semaphore("s_bar"))
        self.barcnt = 0
        self.psn = 0
        self.reset()

    def reset(self):
        self.ops = {e: [] for e in ENGS}
        self.lastw = {}
        self.readers = {}

    def _add(self, eng, rec, reads, writes):
        idx = len(self.ops[eng])
        deps = set()
        for k in list(reads) + list(writes):
            lw = self.lastw.get(k)
            if lw is not None:
                deps.add(lw)
        for k in writes:
            for r in self.readers.get(k, ()):
                deps.add(r)
        deps.discard((eng, idx))
        rec["deps"] = deps
        rec["signal"] = bool(rec["dma"])
        self.ops[eng].append(rec)
        for k in writes:
            self.lastw[k] = (eng, idx)
            self.readers[k] = []
        for k in reads:
            self.readers.setdefault(k, []).append((eng, idx))

    def op(self, eng, fn, reads=(), writes=()):
        self._add(eng, {"fn": fn, "dma": False}, reads, writes)

    def dma(self, q, fns, reads=(), writes=()):
        if not isinstance(fns, (list, tuple)):
            fns = [fns]
        self._add(q, {"fn": list(fns), "dma": True}, reads, writes)

    def emit(self):
        nc = self.nc
        ops = self.ops
        for e in ENGS:
            for rec in ops[e]:
                for (de, di) in rec["deps"]:
                    d = ops[de][di]
                    if de == e and not d["dma"] and e == "pe":
                        continue
                    d["signal"] = True
        for e in ENGS:
            comp = [r for r in ops[e] if not r["dma"]]
            if comp:
                comp[-1]["signal"] = True
        for e in ENGS:
            for rec in ops[e]:
                if not rec["signal"]:
                    continue
                if rec["dma"]:
                    j = self.dnext[e]
                    self.dnext[e] = (j + 1) % len(self.dsems[e])
                    prev = self.dcnt[e][j]
                    self.dcnt[e][j] = prev + 16 * len(rec["fn"])
                    rec["sem"] = self.dsems[e][j]
                    rec["val"] = self.dcnt[e][j]
                    rec["prev"] = prev
                else:
                    self.cnt[e] += 1
                    rec["sem"] = self.sem[e]
                    rec["val"] = self.cnt[e]
        self.barcnt += 1
        barval = self.barcnt * len(ENGS)
        with nc.Block() as block:
            def body(e):
                def f(eng):
                    seen = self.seen[e]

                    def wait(sem, val):
                        key = id(sem)
                        if seen.get(key, 0) >= val:
                            return
                        eng.wait_ge(sem, val)
                        seen[key] = val
                    for rec in ops[e]:
                        for (de, di) in sorted(rec["deps"]):
                            d = ops[de][di]
                            if not d["signal"]:
                                continue
                            if de == e and not d["dma"] and e == "pe":
                                continue
                            wait(d["sem"], d["val"])
                        if rec["dma"]:
                            if rec["prev"] > 0:
                                wait(rec["sem"], rec["prev"])
                            for fn in rec["fn"]:
                                fn(eng).then_inc(rec["sem"], 16)
                        else:
                            ins = rec["fn"](eng)
                            if rec["signal"]:
                                ins.then_inc(rec["sem"], 1)
                    if e in self.dsems:
                        for j, s in enumerate(self.dsems[e]):
                            if self.dcnt[e][j] > 0:
                                wait(s, self.dcnt[e][j])
                    if self.cnt[e] > 0:
                        wait(self.sem[e], self.cnt[e])
                    eng.sem_inc(self.bar, 1)
                    eng.wait_ge(self.bar, barval)
                return f
            block.tensor(body("pe"))
            block.scalar(body("act"))
            block.vector(body("dve"))
            block.gpsimd(body("pool"))
            block.sync(body("sp"))
        self.reset()


def make_consts(T):
    bf = ml_dtypes.bfloat16
    c = {}
    c["ident_f"] = np.eye(128, dtype=np.float32)
    c["ident_b"] = np.eye(128, dtype=np.float32).astype(bf)
    c["ones_b"] = np.ones((128, 128), np.float32).astype(bf)
    slopes = [2.0 ** (-8.0 * (h + 1) / 4) for h in range(4)]
    t = np.arange(T)
    Aq, bq = (t // 128).astype(np.float64), (t % 128).astype(np.float64)
    augq = np.zeros((4, 4, T), np.float32)
    augk = np.zeros((4, 4, T), np.float32)
    for h, s in enumerate(slopes):
        augq[h, 0] = -s * 128 * Aq
        augq[h, 1] = -s * bq
        augq[h, 2] = 1
        augq[h, 3] = 1
        augk[h, 0] = 1
        augk[h, 1] = 1
        augk[h, 2] = s * 128 * Aq
        augk[h, 3] = s * bq
    c["augq"] = augq.astype(bf)
    c["augk"] = augk.astype(bf)
    kk = np.arange(128)[:, None]
    qq = np.arange(128)[None, :]
    cd = np.zeros((128, 4, 128), np.float32)
    for h, s in enumerate(slopes):
        cd[:, h, :] = -2 * s * np.maximum(kk - qq, 0) - 30000.0 * ((kk // 64) > (qq // 64))
    c["cdiag"] = cd
    m0 = np.zeros((128, 128), np.float32)
    m0[64:, :64] = -30000.0
    m4 = np.zeros((128, 128), np.float32)
    m4[:64, 64:] = -30000.0
    c["dmask"] = np.stack([m0, m4], axis=1)
    s64 = np.arange(64)[:, None]
    t64 = np.arange(64)[None, :]
    c["triu"] = np.where(s64 <= t64, -1.0 / 16, 0.0).astype(np.float32)
    c["trisl"] = np.where(s64 > t64, -1.0 / 16, 0.0).astype(np.float32)
    c["mask01"] = np.where(s64 <= t64, 1.0, 0.0).astype(np.float32)
    c["maskneg"] = np.where(s64 > t64, -30000.0, 0.0).astype(np.float32)
    sel = np.zeros((4, 4, 128), np.float32)
    for h in range(4):
        sel[h, h, :] = 1
    c["sel"] = sel
    t128 = np.arange(128)
    c["rmask01"] = np.tile(np.where(t128 % 64 == 0, 0.0, 1.0).astype(np.float32), (128, 1))
    c["rmaskneg"] = np.tile(np.where(t128 % 64 == 0, -1e30, 0.0).astype(np.float32), (128, 1))
    c["onesrow"] = np.ones((1, T), np.float32)
    return c


CONST_SPECS = {"ident_f": F32, "ident_b": BF16, "ones_b": BF16, "augq": BF16, "augk": BF16, "cdiag": F32,
               "dmask": F32, "triu": F32, "trisl": F32, "mask01": F32, "maskneg": F32, "sel": F32,
               "rmask01": F32, "rmaskneg": F32, "onesrow": F32}


def prep_params(inp, L, T):
    f = lambda a: np.ascontiguousarray(np.asarray(a, dtype=np.float32))
    p = {}
    colmaj = lambda a: f(np.asarray(a).reshape(L, DC, 128).transpose(0, 2, 1))
    p["g_mix_pre"] = colmaj(inp["norm_mix_pre"])
    p["g_mix_post"] = colmaj(inp["norm_mix_post"])
    p["g_ffn_pre"] = colmaj(inp["norm_ffn_pre"])
    p["g_ffn_post"] = colmaj(inp["norm_ffn_post"])
    p["b_gate_c"] = f(np.asarray(inp["b_gate"]).reshape(L, 4, DC, 128).transpose(0, 3, 1, 2))
    p["diff_lambda"] = f(np.asarray(inp["diff_lambda"]).reshape(L, 1, 256))
    p["diff_norm"] = f(np.asarray(inp["diff_norm"]).reshape(L, 1, 512))
    p["gla_norm"] = f(np.asarray(inp["gla_norm"]).reshape(L, 1, 512))
    p["mlstm_norm"] = f(np.asarray(inp["mlstm_norm"]).reshape(L, 1, 512))
    wg = np.concatenate([np.asarray(inp["gla_w_gate_up"]), np.asarray(inp["gla_b_gate"])[:, None, :]], axis=1)
    p["gla_wg"] = f(wg)
    p["conv_w"] = f(np.asarray(inp["mlstm_conv_w"]).reshape(L, 4, 8, 128).transpose(0, 3, 2, 1))
    p["conv_b"] = f(np.asarray(inp["mlstm_conv_b"]).reshape(L, 8, 128).transpose(0, 2, 1))
    p["b_i"] = f(np.repeat(np.asarray(inp["mlstm_b_i"]).reshape(L, 4), T // 128, axis=1).reshape(L, 4 * (T // 128), 1))
    p["b_f"] = f(np.repeat(np.asarray(inp["mlstm_b_f"]).reshape(L, 4), T // 128, axis=1).reshape(L, 4 * (T // 128), 1))
    rb = np.asarray(inp["rel_bias"], dtype=np.float32)
    kk = np.arange(128)[:, None]
    qq = np.arange(128)[None, :]
    tiles = []
    for dl in (0, 1):
        idx = np.clip(128 * dl + qq - kk, -128, 128) + 128
        tiles.append(rb[:, :, idx])
    p["d_bias"] = f(np.stack(tiles, axis=2).transpose(0, 3, 2, 1, 4))
    p["d_far"] = f(np.broadcast_to(rb[:, None, :, 256], (L, 128, 4)))
    return p


PARAM_SHAPES = lambda L, T: {
    "g_mix_pre": [L, 128, 16], "g_mix_post": [L, 128, 16], "g_ffn_pre": [L, 128, 16], "g_ffn_post": [L, 128, 16],
    "b_gate_c": [L, 128, 4, 16], "diff_lambda": [L, 1, 256], "diff_norm": [L, 1, 512], "gla_norm": [L, 1, 512],
    "mlstm_norm": [L, 1, 512], "gla_wg": [L, 17, 256], "conv_w": [L, 128, 8, 4], "conv_b": [L, 128, 8],
    "b_i": [L, 4 * (T // 128), 1], "b_f": [L, 4 * (T // 128), 1], "d_bias": [L, 128, 2, 4, 128], "d_far": [L, 128, 4]}

BIGW = lambda L: {"w_in": [L, D, NIN], "w_branch": [L, 4, 512, D], "w_gate": [L, 4, D, D], "w_out": [L, D, D],
                  "w_up": [L, D, DFF], "w_down": [L, DFF, D]}


def build(T, L, dbg=()):
    nc = bass.Bass("TRN2", target_bir_lowering=False)
    NT = T // 512
    NB = T // 128
    NCH = T // 64
    dr = {}
    _uid = [0]

    def U(n):
        _uid[0] += 1
        return f"{n}_{_uid[0]}"

    def din(name, shape, dt):
        dr[name] = nc.dram_tensor(name, list(shape), dt, kind="ExternalInput").ap()
        return dr[name]

    def scr(name, shape, dt):
        kind = "ExternalOutput" if name in dbg else "Internal"
        dr[name] = nc.dram_tensor(name, list(shape), dt, kind=kind).ap()
        return dr[name]

    x_in = din("x", [T, D], F32)
    for k, shp in BIGW(L).items():
        din(k, shp, F32)
    for k, shp in PARAM_SHAPES(L, T).items():
        din(k, shp, F32)
    cs = make_consts(T)
    for k, dt in CONST_SPECS.items():
        din("c_" + k, cs[k].shape, dt)
    out = nc.dram_tensor("out", [T, D], F32, kind="ExternalOutput").ap()

    xT = scr("xT", [128, DC, T], F32)
    yT = scr("yT", [128, DC, T], F32)
    hT = scr("hT", [128, DC, T], BF16)
    mT = scr("mT", [128, DC, T], BF16)
    brT = scr("brT", [128, DC, T], BF16)
    fT = scr("fT", [128, 64, T], BF16)
    gT = scr("gT", [4, 128, DC, T], BF16)
    aq = scr("aq", [8, 64, T], BF16)
    ak = scr("ak", [8, 64, T], BF16)
    av = scr("av", [T, 512], BF16)
    bq = scr("bq", [4, 64, T], F32)
    bk = scr("bk", [4, 64, T], F32)
    bkT = scr("bkT", [T, 256], F32)
    bv = scr("bv", [T, 512], BF16)
    bg = scr("bg", [16, T], F32)
    br = scr("br", [T, 512], F32)
    cqk = scr("cqk", [8, 128, T], F32)
    cqk2 = scr("cqk2", [8, 128, T], BF16)
    cv = scr("cv", [T, 512], BF16)
    co = scr("co", [T, 512], F32)
    cif = scr("cif", [8, T], F32)
    dq = scr("dq", [4, 128, T], BF16)
    dk = scr("dk", [4, 128, T], BF16)
    dv = scr("dv", [T, 512], BF16)
    chd = scr("chd", [3, 4, NCH], F32)
    stkd = scr("stkd", [16, T], F32)
    rtd = scr("rtd", [4, T], F32)

    with ExitStack() as es:
        S = Sched(nc, es)
        ps = [es.enter_context(nc.psum_tensor(f"ps{i}", [128, 512], F32)) for i in range(8)]
        ident_f = es.enter_context(nc.sbuf_tensor(U("ident_f"), [128, 128], F32))
        ident_b = es.enter_context(nc.sbuf_tensor(U("ident_b"), [128, 128], BF16))
        ones_b = es.enter_context(nc.sbuf_tensor(U("ones_b"), [128, 128], BF16))
        S.dma("sp", lambda e: e.dma_start(out=ident_f[:], in_=dr["c_ident_f"]), writes=["ident_f"])
        S.dma("sp", lambda e: e.dma_start(out=ident_b[:], in_=dr["c_ident_b"]), writes=["ident_b"])
        S.dma("sp", lambda e: e.dma_start(out=ones_b[:], in_=dr["c_ones_b"]), writes=["ones_b"])
        S.emit()

        cnt = {"ev": 0}

        def evac_eng():
            cnt["ev"] += 1
            return "act" if cnt["ev"] % 2 == 0 else "dve"

        def next_bank(nb=6):
            i = S.psn % nb
            S.psn += 1
            return i

        def norm_tile(xt, xkey, gcol, tgn, h_out, hkey, sq, rs, psb):
            for c4 in range(4):
                S.op("act", lambda e, c4=c4: e.activation(out=sq[:, 4 * c4:4 * c4 + 4, :], in_=xt[:, 4 * c4:4 * c4 + 4, :], func=AF.Square),
                     reads=[xkey], writes=[("sq", c4)])
            for c in range(DC):
                S.op("pe", lambda e, c=c: e.matmul(ps[psb][:, 0:tgn], ones_b[:], sq[:, c, :], start=(c == 0), stop=(c == DC - 1)),
                     reads=[("sq", c // 4), "ones_b"], writes=[("ps", psb)])
            S.op("act", lambda e: e.activation(out=rs[:, 0:tgn], in_=ps[psb][:, 0:tgn], func=AF.Sqrt, bias=EPS, scale=1.0 / D),
                 reads=[("ps", psb)], writes=["rs"])
            S.op("dve", lambda e: e.reciprocal(out=rs[:, 0:tgn], in_=rs[:, 0:tgn]), reads=["rs"], writes=["rs"])
            for c in range(DC):
                S.op("dve", lambda e, c=c: e.scalar_tensor_tensor(out=h_out[:, c, :], in0=xt[:, c, :], scalar=gcol[:, c:c + 1], in1=rs[:, 0:tgn],
                                                                   op0=ALU.mult, op1=ALU.mult),
                     reads=[xkey, "rs", "gcol"], writes=[hkey])

        def phase_in():
            with ExitStack() as _st:
                xin = _st.enter_context(nc.sbuf_tensor(U("xin"), [128, 2, D], F32))
                xt = _st.enter_context(nc.sbuf_tensor(U("xt"), [128, DC, 512], F32))
                sq = _st.enter_context(nc.sbuf_tensor(U("sq"), [128, DC, 512], BF16))
                rs = _st.enter_context(nc.sbuf_tensor(U("rs"), [128, 512], F32))
                ht = _st.enter_context(nc.sbuf_tensor(U("ht"), [128, DC, 512], BF16))
                gcol = _st.enter_context(nc.sbuf_tensor(U("gcol"), [128, DC], F32))
                S.dma("sp", lambda e: e.dma_start(out=gcol[:], in_=dr["g_mix_pre"][0]), writes=["gcol"])
                for tg in range(NT):
                    for tb in range(4):
                        r0 = tg * 512 + tb * 128
                        sl = (tg * 4 + tb) % 2
                        S.dma("sp", lambda e, r0=r0, sl=sl: e.dma_start(out=xin[:, sl, :], in_=x_in[r0:r0 + 128, :]), writes=[("xin", sl)])
                        for cg in range(4):
                            b = next_bank()
                            for j in range(4):
                                c = cg * 4 + j
                                S.op("pe", lambda e, b=b, j=j, c=c, sl=sl: e.transpose(ps[b][:, 128 * j:128 * j + 128], xin[:, sl, 128 * c:128 * c + 128], ident_f[:]),
                                     reads=[("xin", sl), "ident_f"], writes=[("ps", b)])
                            if evac_eng() == "act":
                                S.op("act", lambda e, b=b, cg=cg, tb=tb: e.activation(out=xt[:, 4 * cg:4 * cg + 4, 128 * tb:128 * tb + 128],
                                                                                     in_=ps[b][:, :].rearrange("p (j t) -> p j t", j=4), func=AF.Copy),
                                     reads=[("ps", b)], writes=["xt"])
                            else:
                                S.op("dve", lambda e, b=b, cg=cg, tb=tb: e.tensor_copy(out=xt[:, 4 * cg:4 * cg + 4, 128 * tb:128 * tb + 128],
                                                                                      in_=ps[b][:, :].rearrange("p (j t) -> p j t", j=4)),
                                     reads=[("ps", b)], writes=["xt"])
                    S.dma("sp", lambda e, tg=tg: e.dma_start(out=xT[:, :, tg * 512:(tg + 1) * 512], in_=xt[:]), reads=["xt"])
                    norm_tile(xt, "xt", gcol, 512, ht, "ht", sq, rs, 7)
                    S.dma("sp", lambda e, tg=tg: e.dma_start(out=hT[:, :, tg * 512:(tg + 1) * 512], in_=ht[:]), reads=["ht"])
                S.emit()

        def gemm(A_dram, KC, TS, wblocks, wb_cols, stf_n=4):
            with ExitStack() as _st:
                A_sb = _st.enter_context(nc.sbuf_tensor(U("A_sb"), [128, KC, TS], BF16))
                wt = _st.enter_context(nc.sbuf_tensor(U("wt"), [128, 2, KC, wb_cols], BF16))
                stF = _st.enter_context(nc.sbuf_tensor(U("stF"), [128, stf_n, 512], F32))
                stB = _st.enter_context(nc.sbuf_tensor(U("stB"), [128, stf_n, 512], BF16))
                stc = [0]

                def loadw(i):
                    w_ap, ncols, _ = wblocks[i]
                    sl = i % 2
                    fns = []
                    step = max(1, KC // 4)
                    for k0 in range(0, KC, step):
                        fns.append(lambda e, k0=k0, sl=sl, w_ap=w_ap, ncols=ncols, step=step: e.dma_start(
                            out=wt[:, sl, k0:k0 + step, 0:ncols],
                            in_=w_ap[k0 * 128:(k0 + step) * 128, :].rearrange("(c p) n -> p c n", p=128)))
                    S.dma("pool", fns, writes=[("w", sl)])
                for ts in range(T // TS):
                    for k in range(KC):
                        S.dma("sp", lambda e, k=k, ts=ts: e.dma_start(out=A_sb[:, k, :], in_=A_dram[:, k, ts * TS:(ts + 1) * TS]), writes=[("A", k)])
                    loadw(0)
                    for i, (w_ap, ncols, jobs) in enumerate(wblocks):
                        if i + 1 < len(wblocks):
                            loadw(i + 1)
                        sl = i % 2
                        for (kind, c0, m, epi) in jobs:
                            if kind == "fm":
                                for tg in range(TS // 512):
                                    b = next_bank()
                                    for k in range(KC):
                                        S.op("pe", lambda e, b=b, k=k, sl=sl, c0=c0, m=m, tg=tg: e.matmul(
                                            ps[b][0:m, 0:512], wt[:, sl, k, c0:c0 + m], A_sb[:, k, tg * 512:(tg + 1) * 512],
                                            start=(k == 0), stop=(k == KC - 1)), reads=[("A", k), ("w", sl)], writes=[("ps", b)])
                                    s_ = stc[0] % stf_n
                                    stc[0] += 1
                                    epi(ps[b][0:m, 0:512], ts * TS + tg * 512, ("ps", b), stF[0:m, s_, :], stB[0:m, s_, :], ("st", s_))
                            else:
                                for tb in range(TS // 128):
                                    b = next_bank()
                                    for k in range(KC):
                                        S.op("pe", lambda e, b=b, k=k, sl=sl, c0=c0, m=m, tb=tb: e.matmul(
                                            ps[b][:, 0:m], A_sb[:, k, tb * 128:(tb + 1) * 128], wt[:, sl, k, c0:c0 + m],
                                            start=(k == 0), stop=(k == KC - 1)), reads=[("A", k), ("w", sl)], writes=[("ps", b)])
                                    s_ = stc[0] % stf_n
                                    stc[0] += 1
                                    epi(ps[b][:, 0:m], ts * TS + tb * 128, ("ps", b), stF[:, s_, 0:m], stB[:, s_, 0:m], ("st", s_))
                S.emit()

        def epi_simple(dst_fn, dt, func=None, scale=None, bias=None, post=None):
            def epi(psap, t0, pskey, stF, stB, skey):
                st = stB if dt == BF16 else stF
                reads = [pskey]
                if bias is not None:
                    reads.append("bias")
                if post == "relu2":
                    S.op("act", lambda e: e.activation(out=stF, in_=psap, func=AF.Relu), reads=reads, writes=[skey])
                    eng = "pool" if (t0 // 512) % 2 == 0 else "dve"
                    S.op(eng, lambda e: e.tensor_tensor(out=stB, in0=stF, in1=stF, op=ALU.mult), reads=[skey], writes=[skey])
                    st = stB
                elif func is not None or bias is not None:
                    kw = {}
                    if bias is not None:
                        kw["bias"] = bias
                    if scale is not None:
                        kw["scale"] = scale
                    S.op("act", lambda e: e.activation(out=st, in_=psap, func=(func or AF.Identity), **kw), reads=reads, writes=[skey])
                else:
                    eng = evac_eng()
                    if eng == "act":
                        S.op("act", lambda e: e.activation(out=st, in_=psap, func=AF.Copy, scale=(1.0 if scale is None else scale)), reads=reads, writes=[skey])
                    elif scale is None:
                        S.op("dve", lambda e: e.tensor_copy(out=st, in_=psap), reads=reads, writes=[skey])
                    else:
                        S.op("dve", lambda e: e.tensor_scalar(out=st, in0=psap, scalar1=float(scale), scalar2=None, op0=ALU.mult), reads=reads, writes=[skey])
                S.dma("sp", lambda e: e.dma_start(out=dst_fn(t0), in_=st), reads=[skey])
            return epi

        def phase_inproj(l):
            w = dr["w_in"][l]
            blocks = []

            def blk(c0, n, jobs):
                blocks.append((w[:, c0:c0 + n], n, jobs))
            fm = lambda dst, m, **kw: epi_simple(lambda t0: dst[:, t0:t0 + 512], **kw)
            tm = lambda dst, n, **kw: epi_simple(lambda t0: dst[t0:t0 + 128, 0:n], **kw)
            blk(A0, 512, [("fm", 64 * j, 64, fm(aq[j], 64, dt=BF16, scale=0.125)) for j in range(8)])
            blk(A0 + 512, 512, [("fm", 64 * j, 64, fm(ak[j], 64, dt=BF16)) for j in range(8)])
            blk(A0 + 1024, 512, [("tm", 0, 512, tm(av, 512, dt=BF16))])
            blk(B0, 512, [("fm", 64 * j, 64, fm(bq[j], 64, dt=F32, scale=0.125)) for j in range(4)] +
                [("fm", 256 + 64 * j, 64, fm(bk[j], 64, dt=F32)) for j in range(4)] +
                [("tm", 256, 256, tm(bkT, 256, dt=F32))])
            blk(B0 + 512, 512, [("tm", 0, 512, tm(bv, 512, dt=BF16))])
            blk(B0 + 1024, 16, [("fm", 0, 16, fm(bg, 16, dt=F32))])
            blk(B0 + 1040, 512, [("tm", 0, 512, tm(br, 512, dt=F32, func=AF.Silu))])
            blk(C0, 512, [("fm", 128 * j, 128, fm(cqk[j], 128, dt=F32)) for j in range(4)])
            blk(C0 + 512, 512, [("fm", 128 * j, 128, fm(cqk[4 + j], 128, dt=F32)) for j in range(4)])
            blk(C0 + 1024, 512, [("tm", 0, 512, tm(cv, 512, dt=BF16))])
            blk(C0 + 1536, 512, [("tm", 0, 512, tm(co, 512, dt=F32, func=AF.Sigmoid))])
            blk(C0 + 2048, 8, [("fm", 0, 8, fm(cif, 8, dt=F32))])
            blk(D0, 512, [("fm", 128 * j, 128, fm(dq[j], 128, dt=BF16, scale=128 ** -0.5)) for j in range(4)])
            blk(D0 + 512, 512, [("fm", 128 * j, 128, fm(dk[j], 128, dt=BF16)) for j in range(4)])
            blk(D0 + 1024, 512, [("tm", 0, 512, tm(dv, 512, dt=BF16))])
            gemm(hT, DC, T, blocks, 512)

        def phase_gates(l):
            with ExitStack() as _st:
                bgc = _st.enter_context(nc.sbuf_tensor(U("bgc"), [128, 4, DC], F32))
                S.dma("sp", lambda e: e.dma_start(out=bgc[:], in_=dr["b_gate_c"][l]), writes=["bias"])
                blocks = []
                for b in range(4):
                    for cb in range(4):
                        jobs = []
                        for j in range(4):
                            c = cb * 4 + j
                            jobs.append(("fm", 128 * j, 128, epi_simple(lambda t0, b=b, c=c: gT[b, :, c, t0:t0 + 512], dt=BF16, func=AF.Sigmoid,
                                                                       bias=bgc[:, b, c:c + 1])))
                        blocks.append((dr["w_gate"][l, b][:, cb * 512:(cb + 1) * 512], 512, jobs))
                gemm(hT, DC, T, blocks, 512)

        def phase_outproj(l):
            blocks = []
            for cb in range(4):
                jobs = [("fm", 128 * j, 128, epi_simple(lambda t0, c=cb * 4 + j: yT[:, c, t0:t0 + 512], dt=F32)) for j in range(4)]
                blocks.append((dr["w_out"][l][:, cb * 512:(cb + 1) * 512], 512, jobs))
            gemm(mT, DC, T, blocks, 512)

        def phase_ffn_up(l):
            blocks = []
            for cb in range(16):
                jobs = [("fm", 128 * j, 128, epi_simple(lambda t0, c=cb * 4 + j: fT[:, c, t0:t0 + 512], dt=BF16, post="relu2")) for j in range(4)]
                blocks.append((dr["w_up"][l][:, cb * 512:(cb + 1) * 512], 512, jobs))
            gemm(hT, DC, T, blocks, 512)

        def phase_ffn_down(l):
            blocks = []
            for c in range(DC):
                jobs = [("fm", 0, 128, epi_simple(lambda t0, c=c: yT[:, c, t0:t0 + 512], dt=F32))]
                blocks.append((dr["w_down"][l][:, c * 128:(c + 1) * 128], 128, jobs))
            gemm(fT, 64, min(T, 1024), blocks, 128)

        def phase_norm(gpost_ap, gnext_ap, final):
            with ExitStack() as _st:
                yt = _st.enter_context(nc.sbuf_tensor(U("yt"), [128, 2, DC, 512], F32))
                xt = _st.enter_context(nc.sbuf_tensor(U("xt"), [128, 2, DC, 512], F32))
                sq = _st.enter_context(nc.sbuf_tensor(U("sq"), [128, DC, 512], BF16))
                rs = _st.enter_context(nc.sbuf_tensor(U("rs"), [128, 512], F32))
                ht = _st.enter_context(nc.sbuf_tensor(U("ht"), [128, DC, 512], BF16))
                gpost = _st.enter_context(nc.sbuf_tensor(U("gpost"), [128, DC], F32))
                gcol = _st.enter_context(nc.sbuf_tensor(U("gcol"), [128, DC], F32))
                xo = _st.enter_context(nc.sbuf_tensor(U("xo"), [128, 2, D], F32))
                S.dma("sp", lambda e: e.dma_start(out=gpost[:], in_=gpost_ap), writes=["gpost"])
                if not final:
                    S.dma("sp", lambda e: e.dma_start(out=gcol[:], in_=gnext_ap), writes=["gcol"])

                def load(tg):
                    sl = tg % 2
                    S.dma("sp", lambda e: e.dma_start(out=yt[:, sl], in_=yT[:, :, tg * 512:(tg + 1) * 512]), writes=[("yt", sl)])
                    S.dma("sp", lambda e: e.dma_start(out=xt[:, sl], in_=xT[:, :, tg * 512:(tg + 1) * 512]), writes=[("xt", sl)])
                load(0)
                for tg in range(NT):
                    sl = tg % 2
                    if tg + 1 < NT:
                        load(tg + 1)
                    y = yt[:, sl]
                    xx = xt[:, sl]
                    for c4 in range(4):
                        S.op("act", lambda e, c4=c4, y=y: e.activation(out=sq[:, 4 * c4:4 * c4 + 4, :], in_=y[:, 4 * c4:4 * c4 + 4, :], func=AF.Square),
                             reads=[("yt", sl)], writes=[("sq", c4)])
                    for c in range(DC):
                        S.op("pe", lambda e, c=c: e.matmul(ps[6][:, 0:512], ones_b[:], sq[:, c, :], start=(c == 0), stop=(c == DC - 1)),
                             reads=[("sq", c // 4), "ones_b"], writes=[("ps", 6)])
                    S.op("act", lambda e: e.activation(out=rs[:], in_=ps[6][:, 0:512], func=AF.Sqrt, bias=EPS, scale=1.0 / D), reads=[("ps", 6)], writes=["rs"])
                    S.op("dve", lambda e: e.reciprocal(out=rs[:], in_=rs[:]), reads=["rs"], writes=["rs"])
                    for c in range(DC):
                        S.op("dve", lambda e, c=c, y=y: e.scalar_tensor_tensor(out=y[:, c, :], in0=y[:, c, :], scalar=gpost[:, c:c + 1], in1=rs[:],
                                                                               op0=ALU.mult, op1=ALU.mult), reads=[("yt", sl), "rs", "gpost"], writes=[("yt", sl)])
                        S.op("pool", lambda e, c=c, y=y, xx=xx: e.tensor_tensor(out=xx[:, c, :], in0=xx[:, c, :], in1=y[:, c, :], op=ALU.add),
                             reads=[("yt", sl), ("xt", sl)], writes=[("xt", sl)])
                    if not final:
                        S.dma("sp", lambda e, tg=tg, xx=xx: e.dma_start(out=xT[:, :, tg * 512:(tg + 1) * 512], in_=xx), reads=[("xt", sl)])
                        norm_tile(xx, ("xt", sl), gcol, 512, ht, "ht", sq, rs, 7)
                        S.dma("sp", lambda e, tg=tg: e.dma_start(out=hT[:, :, tg * 512:(tg + 1) * 512], in_=ht[:]), reads=["ht"])
                    else:
                        for tb in range(4):
                            osl = tb % 2
                            for cg in range(4):
                                b = next_bank()
                                for j in range(4):
                                    c = cg * 4 + j
                                    S.op("pe", lambda e, b=b, j=j, c=c, tb=tb, xx=xx: e.transpose(ps[b][:, 128 * j:128 * j + 128], xx[:, c, 128 * tb:128 * tb + 128], ident_f[:]),
                                         reads=[("xt", sl), "ident_f"], writes=[("ps", b)])
                                eng = evac_eng()
                                if eng == "act":
                                    S.op("act", lambda e, b=b, cg=cg, osl=osl: e.activation(out=xo[:, osl, 512 * cg:512 * cg + 512], in_=ps[b][:, :], func=AF.Copy),
                                         reads=[("ps", b)], writes=[("xo", osl)])
                                else:
                                    S.op("dve", lambda e, b=b, cg=cg, osl=osl: e.tensor_copy(out=xo[:, osl, 512 * cg:512 * cg + 512], in_=ps[b][:, :]),
                                         reads=[("ps", b)], writes=[("xo", osl)])
                            r0 = tg * 512 + tb * 128
                            S.dma("sp", lambda e, r0=r0, osl=osl: e.dma_start(out=out[r0:r0 + 128, :], in_=xo[:, osl, :]), reads=[("xo", osl)])
                S.emit()

        def phase_merge(l):
            with ExitStack() as _st:
                wb = _st.enter_context(nc.sbuf_tensor(U("wb"), [128, 4, 4, D], BF16))
                brs = _st.enter_context(nc.sbuf_tensor(U("brs"), [128, 2, DC, 512], BF16))
                gs = _st.enter_context(nc.sbuf_tensor(U("gs"), [128, 2, 4, 4, 512], BF16))
                tt = _st.enter_context(nc.sbuf_tensor(U("tt"), [128, 2, 4, 512], F32))
                mt = _st.enter_context(nc.sbuf_tensor(U("mt"), [128, DC, 512], BF16))
                for b in range(4):
                    S.dma("pool", [lambda e, b=b, n0=n0: e.dma_start(out=wb[:, b, :, n0:n0 + 512], in_=dr["w_branch"][l, b][:, n0:n0 + 512].rearrange("(c p) n -> p c n", p=128))
                                   for n0 in range(0, D, 512)], writes=[("wb", b)])

                def loadbr(tg):
                    S.dma("sp", lambda e: e.dma_start(out=brs[:, tg % 2], in_=brT[:, :, tg * 512:(tg + 1) * 512]), writes=[("brs", tg % 2)])

                def loadg(i):
                    tg, cg = divmod(i, 4)
                    sl = i % 2
                    S.dma("sp", [lambda e, b=b: e.dma_start(out=gs[:, sl, b], in_=gT[b, :, 4 * cg:4 * cg + 4, tg * 512:(tg + 1) * 512]) for b in range(4)],
                          writes=[("gs", sl)])
                loadbr(0)
                loadg(0)
                for tg in range(NT):
                    if tg + 1 < NT:
                        loadbr(tg + 1)
                    for cg in range(4):
                        i = tg * 4 + cg
                        if i + 1 < NT * 4:
                            loadg(i + 1)
                        for j in range(4):
                            c = cg * 4 + j
                            pp = c % 2
                            for b in range(4):
                                bk = 4 * pp + b
                                for k in range(4):
                                    S.op("pe", lambda e, b=b, bk=bk, k=k, c=c, tg=tg: e.matmul(ps[bk][:, 0:512], wb[:, b, k, 128 * c:128 * c + 128], brs[:, tg % 2, 4 * b + k, :],
                                                                                       start=(k == 0), stop=(k == 3)),
                                         reads=[("wb", b), ("brs", tg % 2)], writes=[("ps", bk)])
                                S.op("dve", lambda e, b=b, bk=bk, j=j, i=i, pp=pp: e.tensor_tensor(out=tt[:, pp, b, :], in0=ps[bk][:, 0:512], in1=gs[:, i % 2, b, j, :], op=ALU.mult),
                                     reads=[("ps", bk), ("gs", i % 2)], writes=[("tt", pp, b)])
                            S.op("dve", lambda e, pp=pp: e.tensor_tensor(out=tt[:, pp, 0, :], in0=tt[:, pp, 0, :], in1=tt[:, pp, 1, :], op=ALU.add), reads=[("tt", pp, 0), ("tt", pp, 1)], writes=[("tt", pp, 0)])
                            S.op("pool", lambda e, pp=pp: e.tensor_tensor(out=tt[:, pp, 2, :], in0=tt[:, pp, 2, :], in1=tt[:, pp, 3, :], op=ALU.add), reads=[("tt", pp, 2), ("tt", pp, 3)], writes=[("tt", pp, 2)])
                            S.op("pool", lambda e, c=c, pp=pp: e.tensor_tensor(out=mt[:, c, :], in0=tt[:, pp, 0, :], in1=tt[:, pp, 2, :], op=ALU.add), reads=[("tt", pp, 0), ("tt", pp, 2)], writes=["mt"])
                    S.dma("sp", lambda e, tg=tg: e.dma_start(out=mT[:, :, tg * 512:(tg + 1) * 512], in_=mt[:]), reads=["mt"])
                S.emit()

        def head_norm(x, xkey, P, gain, gkey, outb, okey, sqt, ss, tag):
            S.op("dve", lambda e: e.tensor_tensor(out=sqt, in0=x, in1=x, op=ALU.mult), reads=[xkey], writes=[tag + "sq"])
            S.op("dve", lambda e: e.tensor_reduce(out=ss, in_=sqt, axis=AX.X, op=ALU.add), reads=[tag + "sq"], writes=[tag + "ss"])
            S.op("act", lambda e: e.activation(out=ss, in_=ss, func=AF.Ln, bias=EPS, scale=1.0 / 128), reads=[tag + "ss"], writes=[tag + "ss"])
            S.op("act", lambda e: e.activation(out=ss, in_=ss, func=AF.Exp, scale=-0.5), reads=[tag + "ss"], writes=[tag + "ss"])
            S.op("dve", lambda e: e.tensor_tensor(out=x, in0=x, in1=ss.unsqueeze(2).to_broadcast([P, 4, 128]), op=ALU.mult), reads=[xkey, tag + "ss"], writes=[xkey])
            S.op("dve", lambda e: e.tensor_tensor(out=outb, in0=x, in1=gain, op=ALU.mult), reads=[xkey, gkey], writes=[okey])

        def mixer_D(l):
            with ExitStack() as _st:
                q_sb = _st.enter_context(nc.sbuf_tensor(U("q_sb"), [128, 2, T], BF16))
                k_sb = _st.enter_context(nc.sbuf_tensor(U("k_sb"), [128, 2, T], BF16))
                v_sb = _st.enter_context(nc.sbuf_tensor(U("v_sb"), [128, NB, 4, 130], BF16))
                pT = _st.enter_context(nc.sbuf_tensor(U("pT"), [128, 2, 8, 512], BF16))
                bt = _st.enter_context(nc.sbuf_tensor(U("bt"), [128, 3, 4, 128], F32))
                dmk = _st.enter_context(nc.sbuf_tensor(U("dmk"), [128, 2, 128], F32))
                bfar = _st.enter_context(nc.sbuf_tensor(U("bfar"), [128, 4], F32))
                tmp = _st.enter_context(nc.sbuf_tensor(U("tmp"), [128, 4, 128], F32))
                rc = _st.enter_context(nc.sbuf_tensor(U("rc"), [128, 4], F32))
                o_sb = _st.enter_context(nc.sbuf_tensor(U("o_sb"), [128, 2, 4, 128], BF16))
                stg = _st.enter_context(nc.sbuf_tensor(U("stg"), [128, 2, 512], BF16))
                S.dma("sp", lambda e: e.dma_start(out=bt[:, 0:2], in_=dr["d_bias"][l]), writes=["bt"])
                S.dma("sp", lambda e: e.dma_start(out=dmk[:], in_=dr["c_dmask"]), writes=["dmk"])
                S.dma("sp", lambda e: e.dma_start(out=bfar[:], in_=dr["d_far"][l]), writes=["bfar"])
                S.op("pool", lambda e: e.memset(v_sb[:, :, :, 128:130], 1.0), writes=["v1"])
                S.dma("sp", [lambda e, b0=b0: e.dma_start(out=v_sb[:, b0, :, 0:128], in_=dv[b0 * 128:(b0 + 1) * 128, :].rearrange("p (h d) -> p h d", h=4))
                             for b0 in range(NB)], writes=["v"])
                for h in range(4):
                    S.op("dve", lambda e, h=h: e.tensor_tensor(out=bt[:, 0, h, :], in0=bt[:, 0, h, :], in1=dmk[:, 0, :], op=ALU.add), reads=["bt", "dmk"], writes=["bt"])
                    S.op("dve", lambda e, h=h: e.tensor_scalar(out=bt[:, 2, h, :], in0=dmk[:, 1, :], scalar1=bfar[:, h:h + 1], scalar2=None, op0=ALU.add),
                         reads=["dmk", "bfar"], writes=["bt"])

                def loadqk(h):
                    S.dma("sp", lambda e: e.dma_start(out=q_sb[:, h % 2, :], in_=dq[h]), writes=[("q", h % 2)])
                    S.dma("sp", lambda e: e.dma_start(out=k_sb[:, h % 2, :], in_=dk[h]), writes=[("k", h % 2)])
                loadqk(0)
                it = 0
                for h in range(4):
                    if h + 1 < 4:
                        loadqk(h + 1)
                    hs = h % 2
                    for sb in range(NT):
                        Q0 = sb * 512
                        pb = it % 2
                        it += 1
                        for j in range(8):
                            K0 = Q0 - 512 + 128 * j
                            if K0 < 0:
                                continue
                            ilo, ihi = max(0, j - 4), min(3, j)
                            c0, c1 = 128 * ilo, 128 * (ihi + 1)
                            b = next_bank(3)
                            S.op("pe", lambda e, b=b, K0=K0, c0=c0, c1=c1, Q0=Q0, hs=hs: e.matmul(ps[b][:, c0:c1], k_sb[:, hs, K0:K0 + 128], q_sb[:, hs, Q0 + c0:Q0 + c1],
                                                                                               start=True, stop=True),
                                 reads=[("q", hs), ("k", hs)], writes=[("ps", b)])
                            i = ilo
                            while i <= ihi:
                                dl = i + 4 - j
                                if dl in (0, 1, 4):
                                    bi = {0: 0, 1: 1, 4: 2}[dl]
                                    S.op("dve", lambda e, b=b, i=i, bi=bi, h=h: e.tensor_tensor(out=tmp[:, i, :], in0=ps[b][:, 128 * i:128 * i + 128], in1=bt[:, bi, h, :], op=ALU.add),
                                         reads=[("ps", b), "bt"], writes=[("tmp", i)])
                                    S.op("act", lambda e, i=i, pb=pb, j=j: e.activation(out=pT[:, pb, j, 128 * i:128 * i + 128], in_=tmp[:, i, :], func=AF.Exp),
                                         reads=[("tmp", i)], writes=[("pT", pb, j, i)])
                                    i += 1
                                else:
                                    i2 = i
                                    while i2 + 1 <= ihi and (i2 + 1 + 4 - j) in (2, 3):
                                        i2 += 1
                                    S.op("act", lambda e, b=b, i=i, i2=i2, pb=pb, j=j, h=h: e.activation(out=pT[:, pb, j, 128 * i:128 * (i2 + 1)], in_=ps[b][:, 128 * i:128 * (i2 + 1)],
                                                                                                         func=AF.Exp, bias=bfar[:, h:h + 1]),
                                         reads=[("ps", b), "bfar"], writes=[("pT", pb, j, ii) for ii in range(i, i2 + 1)])
                                    i = i2 + 1
                        for i in range(4):
                            js = [j for j in range(8) if 0 <= i + 4 - j <= 4 and Q0 - 512 + 128 * j >= 0]
                            pvb = 3 + i
                            for n_, j in enumerate(js):
                                kb = (Q0 - 512 + 128 * j) // 128
                                S.op("pe", lambda e, pvb=pvb, pb=pb, j=j, i=i, kb=kb, h=h, n_=n_, js=js: e.matmul(
                                    ps[pvb][:, 0:129], pT[:, pb, j, 128 * i:128 * i + 128], v_sb[:, kb, h, 0:129], start=(n_ == 0), stop=(n_ == len(js) - 1)),
                                    reads=[("pT", pb, j, i), "v", "v1"], writes=[("ps", pvb)])
                            S.op("dve", lambda e, pvb=pvb, i=i: e.reciprocal(out=rc[:, i:i + 1], in_=ps[pvb][:, 128:129]), reads=[("ps", pvb)], writes=[("rc", i)])
                            S.op("act", lambda e, pvb=pvb, i=i, pb=pb: e.activation(out=o_sb[:, pb, i, :], in_=ps[pvb][:, 0:128], func=AF.Identity, scale=rc[:, i:i + 1]),
                                 reads=[("ps", pvb), ("rc", i)], writes=[("o", pb, i)])
                        tb_ = ps[7][:, 0:256].bitcast(BF16)
                        for i in range(4):
                            S.op("pe", lambda e, i=i, pb=pb: e.transpose(tb_[:, 128 * i:128 * i + 128], o_sb[:, pb, i, :], ident_b[:]),
                                 reads=[("o", pb, i), "ident_b"], writes=[("ps", 7)])
                        S.op("dve", lambda e, pb=pb: e.tensor_copy(out=stg[:, pb, :], in_=tb_), reads=[("ps", 7)], writes=[("stg", pb)])
                        S.dma("sp", lambda e, pb=pb, h=h, Q0=Q0: e.dma_start(out=brT[:, 12 + h, Q0:Q0 + 512], in_=stg[:, pb, :]), reads=[("stg", pb)])
                S.emit()

        def mixer_A(l):
            lam_init = 0.8 - 0.6 * math.exp(-0.3 * l)
            with ExitStack() as _st:
                qa = _st.enter_context(nc.sbuf_tensor(U("qa"), [68, 2, 2, T], BF16))
                ka = _st.enter_context(nc.sbuf_tensor(U("ka"), [68, 2, 2, T], BF16))
                v_sb = _st.enter_context(nc.sbuf_tensor(U("v_sb"), [128, NB, 4, 130], BF16))
                pT = _st.enter_context(nc.sbuf_tensor(U("pT"), [128, 3, 512], BF16))
                cdg = _st.enter_context(nc.sbuf_tensor(U("cdg"), [128, 4, 128], F32))
                tmp = _st.enter_context(nc.sbuf_tensor(U("tmp"), [128, 2, 128], F32))
                rc = _st.enter_context(nc.sbuf_tensor(U("rc"), [128, 4], F32))
                om = _st.enter_context(nc.sbuf_tensor(U("om"), [128, 2, 4, 128], F32))
                lam = _st.enter_context(nc.sbuf_tensor(U("lam"), [128, 256], F32))
                lsc = _st.enter_context(nc.sbuf_tensor(U("lsc"), [128, 4], F32))
                gain = _st.enter_context(nc.sbuf_tensor(U("gain"), [128, 512], F32))
                sqt = _st.enter_context(nc.sbuf_tensor(U("sqt"), [128, 4, 128], F32))
                ss = _st.enter_context(nc.sbuf_tensor(U("ss"), [128, 4], F32))
                o_bf = _st.enter_context(nc.sbuf_tensor(U("o_bf"), [128, 4, 128], BF16))
                stg = _st.enter_context(nc.sbuf_tensor(U("stg"), [128, 2, 512], BF16))
                S.dma("sp", lambda e: e.dma_start(out=cdg[:], in_=dr["c_cdiag"]), writes=["cdg"])
                S.dma("sp", lambda e: e.dma_start(out=lam[:], in_=dr["diff_lambda"][l, 0].partition_broadcast(128)), writes=["lam"])
                S.dma("sp", lambda e: e.dma_start(out=gain[:], in_=dr["diff_norm"][l, 0].partition_broadcast(128)), writes=["gain"])
                S.op("pool", lambda e: e.memset(v_sb[:, :, :, 128:130], 1.0), writes=["v1"])
                S.dma("sp", [lambda e, b0=b0: e.dma_start(out=v_sb[:, b0, :, 0:128], in_=av[b0 * 128:(b0 + 1) * 128, :].rearrange("p (h d) -> p h d", h=4))
                             for b0 in range(NB)], writes=["v"])
                S.op("dve", lambda e: e.tensor_tensor(out=lam[:, 0:64], in0=lam[:, 0:64], in1=lam[:, 64:128], op=ALU.mult), reads=["lam"], writes=["lam"])
                S.op("dve", lambda e: e.tensor_tensor(out=lam[:, 128:192], in0=lam[:, 128:192], in1=lam[:, 192:256], op=ALU.mult), reads=["lam"], writes=["lam"])
                S.op("dve", lambda e: e.tensor_reduce(out=lsc[:, 0:2], in_=lam[:, :].rearrange("p (a b) -> p a b", a=2)[:, :, 0:64], axis=AX.X, op=ALU.add),
                     reads=["lam"], writes=["lsc"])
                S.op("act", lambda e: e.activation(out=lsc[:, 0:2], in_=lsc[:, 0:2], func=AF.Exp), reads=["lsc"], writes=["lsc"])
                S.op("dve", lambda e: e.scalar_tensor_tensor(out=lsc[:, 2:3], in0=lsc[:, 1:2], scalar=-lam_init, in1=lsc[:, 0:1], op0=ALU.add, op1=ALU.subtract),
                     reads=["lsc"], writes=["lsc"])
                S.op("dve", lambda e: e.tensor_scalar(out=gain[:], in0=gain[:], scalar1=float(1.0 - lam_init), scalar2=None, op0=ALU.mult), reads=["gain"], writes=["gain"])

                def loadqk(h):
                    hb = h % 2
                    for m in range(2):
                        S.dma("sp", [lambda e, m=m: e.dma_start(out=qa[0:64, hb, m, :], in_=aq[2 * h + m]),
                                     lambda e, m=m: e.dma_start(out=qa[64:68, hb, m, :], in_=dr["c_augq"][h]),
                                     lambda e, m=m: e.dma_start(out=ka[0:64, hb, m, :], in_=ak[2 * h + m]),
                                     lambda e, m=m: e.dma_start(out=ka[64:68, hb, m, :], in_=dr["c_augk"][h])], writes=[("qk", hb, m)])
                loadqk(0)
                LA = 2
                qkb = [0, 1, 7]
                st_ = {"n": 0, "it": 0, "pend": None, "cd": 0}
                slot_of = {}

                def qk_exp(h, hb, sb, m, kb):
                    Q0 = sb * 512
                    K0 = 128 * kb
                    n = st_["n"]
                    st_["n"] += 1
                    b = qkb[n % 3]
                    psl = n % 3
                    slot_of[(h, sb, m, kb)] = psl
                    if K0 < Q0:
                        S.op("pe", lambda e: e.matmul(ps[b][:, 0:512], ka[:, hb, m, K0:K0 + 128], qa[:, hb, m, Q0:Q0 + 512], start=True, stop=True),
                             reads=[("qk", hb, m)], writes=[("ps", b)])
                        S.op("act", lambda e: e.activation(out=pT[:, psl, :], in_=ps[b][:, 0:512], func=AF.Exp), reads=[("ps", b)], writes=[("pT", psl)])
                    else:
                        jl = (K0 - Q0) // 128
                        c0 = 128 * jl
                        S.op("pe", lambda e: e.matmul(ps[b][:, c0:512], ka[:, hb, m, K0:K0 + 128], qa[:, hb, m, Q0 + c0:Q0 + 512], start=True, stop=True),
                             reads=[("qk", hb, m)], writes=[("ps", b)])
                        ts_ = st_["it"] % 2
                        st_["it"] += 1
                        S.op("dve", lambda e: e.tensor_tensor(out=tmp[:, ts_, :], in0=ps[b][:, c0:c0 + 128], in1=cdg[:, h, :], op=ALU.add),
                             reads=[("ps", b), "cdg"], writes=[("tmp", ts_)])
                        S.op("act", lambda e: e.activation(out=pT[:, psl, c0:c0 + 128], in_=tmp[:, ts_, :], func=AF.Exp),
                             reads=[("tmp", ts_)], writes=[("pT", psl)])
                        if jl < 3:
                            S.op("act", lambda e: e.activation(out=pT[:, psl, c0 + 128:512], in_=ps[b][:, c0 + 128:512], func=AF.Exp),
                                 reads=[("ps", b)], writes=[("pT", psl)])

                def pe_tail(h, sb):
                    Q0 = sb * 512
                    tb_ = ps[6][:, 0:256].bitcast(BF16)
                    for i in range(4):
                        S.op("pe", lambda e, i=i: e.transpose(tb_[:, 128 * i:128 * i + 128], o_bf[:, i, :], ident_b[:]), reads=["o_bf", "ident_b"], writes=[("ps", 6)])
                    sg = (h * NT + sb) % 2
                    S.op("dve", lambda e: e.tensor_copy(out=stg[:, sg, :], in_=tb_), reads=[("ps", 6)], writes=[("stg", sg)])
                    S.dma("sp", lambda e: e.dma_start(out=brT[:, h, Q0:Q0 + 512], in_=stg[:, sg, :]), reads=[("stg", sg)])

                def pv(h, hb, sb, m, kb):
                    Q0 = sb * 512
                    K0 = 128 * kb
                    psl = slot_of.pop((h, sb, m, kb))
                    jl = 0 if K0 < Q0 else (K0 - Q0) // 128
                    for i in range(jl, 4):
                        S.op("pe", lambda e, i=i: e.matmul(ps[2 + i][:, 0:129], pT[:, psl, 128 * i:128 * i + 128], v_sb[:, kb, h, 0:129],
                                                          start=(kb == 0), stop=(kb == 4 * sb + i)),
                             reads=[("pT", psl), "v", "v1"], writes=[("ps", 2 + i)])
                    if kb != 4 * sb + 3:
                        return
                    for i in range(4):
                        S.op("dve", lambda e, i=i: e.reciprocal(out=rc[:, i:i + 1], in_=ps[2 + i][:, 128:129]), reads=[("ps", 2 + i)], writes=[("rc", i)])
                        if m == 0:
                            S.op("act", lambda e, i=i: e.activation(out=om[:, 0, i, :], in_=ps[2 + i][:, 0:128], func=AF.Identity, scale=rc[:, i:i + 1]),
                                 reads=[("ps", 2 + i), ("rc", i)], writes=[("om", 0)])
                        else:
                            S.op("dve", lambda e, i=i: e.tensor_scalar(out=om[:, 1, i, :], in0=ps[2 + i][:, 0:128], scalar1=rc[:, i:i + 1], scalar2=lsc[:, 2:3],
                                                                       op0=ALU.mult, op1=ALU.mult), reads=[("ps", 2 + i), ("rc", i), "lsc"], writes=[("om", 1)])
                    if m == 1:
                        S.op("pool", lambda e: e.tensor_tensor(out=om[:, 0], in0=om[:, 0], in1=om[:, 1], op=ALU.add), reads=[("om", 0), ("om", 1)], writes=[("om", 0)])
                        head_norm(om[:, 0], ("om", 0), 128, gain[:, 128 * h:128 * h + 128].unsqueeze(1).to_broadcast([128, 4, 128]), "gain", o_bf[:], "o_bf", sqt[:], ss[:], "A")
                        st_["pend"] = (h, sb)
                        st_["cd"] = 3

                for h in range(4):
                    if h + 1 < 4:
                        loadqk(h + 1)
                    hb = h % 2
                    items = [(sb, m, kb) for sb in range(NT) for m in range(2) for kb in range(4 * sb + 4)]
                    for j in range(min(LA, len(items))):
                        qk_exp(h, hb, *items[j])
                    for idx, itm in enumerate(items):
                        if idx + LA < len(items):
                            qk_exp(h, hb, *items[idx + LA])
                        pv(h, hb, *itm)
                        if st_["pend"] is not None:
                            if st_["cd"] == 0 or idx == len(items) - 1:
                                pe_tail(*st_["pend"])
                                st_["pend"] = None
                            else:
                                st_["cd"] -= 1
                S.emit()

        def mixer_B(l):
            TS = min(T, 1024)
            NCS = TS // 64
            with ExitStack() as _st:
                q_sb = _st.enter_context(nc.sbuf_tensor(U("q_sb"), [64, 4, TS], F32))
                k_sb = _st.enter_context(nc.sbuf_tensor(U("k_sb"), [64, 4, TS], F32))
                kT_sb = _st.enter_context(nc.sbuf_tensor(U("kT_sb"), [64, NCS, 256], F32))
                v_sb = _st.enter_context(nc.sbuf_tensor(U("v_sb"), [64, NCS, 512], BF16))
                r_sb = _st.enter_context(nc.sbuf_tensor(U("r_sb"), [64, NCS, 512], F32))
                g_sb = _st.enter_context(nc.sbuf_tensor(U("g_sb"), [17, TS], F32))
                wg = _st.enter_context(nc.sbuf_tensor(U("wg"), [17, 256], F32))
                cst = _st.enter_context(nc.sbuf_tensor(U("cst"), [64, 3, 64], F32))
                gain = _st.enter_context(nc.sbuf_tensor(U("gain"), [64, 512], F32))
                e1 = _st.enter_context(nc.sbuf_tensor(U("e1"), [64, 256], F32))
                la = _st.enter_context(nc.sbuf_tensor(U("la"), [64, 256], F32))
                kte = _st.enter_context(nc.sbuf_tensor(U("kte"), [64, 256], F32))
                ktl = _st.enter_context(nc.sbuf_tensor(U("ktl"), [64, 2, 256], BF16))
                ecum = _st.enter_context(nc.sbuf_tensor(U("ecum"), [64, 2, 4, 64], F32))
                eneg = _st.enter_context(nc.sbuf_tensor(U("eneg"), [64, 4, 64], F32))
                qd = _st.enter_context(nc.sbuf_tensor(U("qd"), [64, 2, 4, 64], BF16))
                ki = _st.enter_context(nc.sbuf_tensor(U("ki"), [64, 4, 64], BF16))
                att = _st.enter_context(nc.sbuf_tensor(U("att"), [64, 2, 4, 64], BF16))
                Sf = _st.enter_context(nc.sbuf_tensor(U("Sf"), [64, 4, 128], F32))
                Sb = _st.enter_context(nc.sbuf_tensor(U("Sb"), [64, 4, 128], BF16))
                of = _st.enter_context(nc.sbuf_tensor(U("of"), [64, 4, 128], F32))
                gr = _st.enter_context(nc.sbuf_tensor(U("gr"), [64, 2, 4, 128], F32))
                sqt = _st.enter_context(nc.sbuf_tensor(U("sqt"), [64, 4, 128], F32))
                ss = _st.enter_context(nc.sbuf_tensor(U("ss"), [64, 4], F32))
                o_bf = _st.enter_context(nc.sbuf_tensor(U("o_bf"), [64, 4, 128], BF16))
                stg = _st.enter_context(nc.sbuf_tensor(U("stg"), [128, 2, 4, 512], BF16))
                S.dma("sp", lambda e: e.dma_start(out=wg[:], in_=dr["gla_wg"][l]), writes=["wg"])
                S.dma("sp", [lambda e: e.dma_start(out=cst[:, 0, :], in_=dr["c_triu"]), lambda e: e.dma_start(out=cst[:, 1, :], in_=dr["c_trisl"]),
                             lambda e: e.dma_start(out=cst[:, 2, :], in_=dr["c_mask01"])], writes=["cst"])
                S.dma("sp", lambda e: e.dma_start(out=gain[:], in_=dr["gla_norm"][l, 0].partition_broadcast(64)), writes=["gain"])
                S.op("dve", lambda e: e.memset(Sf[:], 0.0), writes=["Sf"])
                S.op("pool", lambda e: e.memset(Sb[:], 0.0), writes=["Sb"])
                for ts in range(T // TS):
                    t0s = ts * TS
                    S.dma("sp", [lambda e, h=h, t0s=t0s: e.dma_start(out=q_sb[:, h, :], in_=bq[h][:, t0s:t0s + TS]) for h in range(4)] +
                          [lambda e, h=h, t0s=t0s: e.dma_start(out=k_sb[:, h, :], in_=bk[h][:, t0s:t0s + TS]) for h in range(4)], writes=["qk"])
                    S.dma("sp", lambda e, t0s=t0s: e.dma_start(out=kT_sb[:], in_=bkT[t0s:t0s + TS, :].rearrange("(c p) n -> p c n", p=64)), writes=["kT"])
                    S.dma("sp", lambda e, t0s=t0s: e.dma_start(out=v_sb[:], in_=bv[t0s:t0s + TS, :].rearrange("(c p) n -> p c n", p=64)), writes=["v"])
                    S.dma("sp", lambda e, t0s=t0s: e.dma_start(out=r_sb[:], in_=br[t0s:t0s + TS, :].rearrange("(c p) n -> p c n", p=64)), writes=["r"])
                    S.dma("sp", [lambda e, t0s=t0s: e.dma_start(out=g_sb[0:16, :], in_=bg[:, t0s:t0s + TS]),
                                 lambda e: e.dma_start(out=g_sb[16:17, :], in_=dr["c_onesrow"][:, 0:TS])], writes=["g"])
                    def b_stage1(c):
                        t0 = c * 64
                        p = c % 2
                        S.op("pe", lambda e: e.matmul(ps[0][0:64, 0:256], g_sb[:, t0:t0 + 64], wg[:], start=True, stop=True), reads=["g", "wg"], writes=[("ps", 0)])
                        S.op("act", lambda e: e.activation(out=e1[:], in_=ps[0][0:64, 0:256], func=AF.Exp, scale=-1.0), reads=[("ps", 0)], writes=["e1"])
                        S.op("act", lambda e: e.activation(out=la[:], in_=e1[:], func=AF.Ln, bias=1.0), reads=["e1"], writes=["la"])
                        S.op("pe", lambda e: e.matmul(ps[1][0:64, 0:256], cst[:, 1, :], la[:], start=True, stop=True), reads=["la", "cst"], writes=[("ps", 1)])
                        for h in range(4):
                            S.op("pe", lambda e, h=h: e.matmul(ps[2][0:64, 64 * h:64 * h + 64], la[:, 64 * h:64 * h + 64], cst[:, 0, :], start=True, stop=True),
                                 reads=["la", "cst"], writes=[("ps", 2)])
                        S.op("act", lambda e: e.activation(out=kte[:], in_=ps[1][0:64, 0:256], func=AF.Exp), reads=[("ps", 1)], writes=["kte"])
                        S.op("dve", lambda e: e.tensor_tensor(out=ktl[:, p, :], in0=kT_sb[:, c, :], in1=kte[:], op=ALU.mult), reads=["kT", "kte"], writes=[("ktl", p)])
                        S.op("act", lambda e: e.activation(out=ecum[:, p], in_=ps[2][0:64, 0:256].rearrange("p (h t) -> p h t", h=4), func=AF.Exp), reads=[("ps", 2)], writes=[("ecum", p)])
                        S.op("act", lambda e: e.activation(out=eneg[:], in_=ps[2][0:64, 0:256].rearrange("p (h t) -> p h t", h=4), func=AF.Exp, scale=-1.0),
                             reads=[("ps", 2)], writes=["eneg"])
                        S.op("dve", lambda e: e.tensor_tensor(out=qd[:, p], in0=q_sb[:, :, t0:t0 + 64], in1=ecum[:, p], op=ALU.mult), reads=["qk", ("ecum", p)], writes=[("qd", p)])
                        S.op("pool", lambda e: e.tensor_tensor(out=ki[:], in0=k_sb[:, :, t0:t0 + 64], in1=eneg[:], op=ALU.mult), reads=["qk", "eneg"], writes=["ki"])
                        for h in range(4):
                            S.op("pe", lambda e, h=h: e.matmul(ps[3][0:64, 64 * h:64 * h + 64], ki[:, h, :], qd[:, p, h, :], start=True, stop=True), reads=["ki", ("qd", p)], writes=[("ps", 3)])
                        S.op("dve", lambda e: e.tensor_tensor(out=att[:, p], in0=ps[3][0:64, 0:256].rearrange("p (h t) -> p h t", h=4),
                                                              in1=cst[:, 2, :].unsqueeze(1).to_broadcast([64, 4, 64]), op=ALU.mult), reads=[("ps", 3), "cst"], writes=[("att", p)])
                        S.op("pool", lambda e: e.tensor_tensor(out=gr[:, p], in0=r_sb[:, c, :].rearrange("p (h d) -> p h d", h=4), in1=gain[:, :].rearrange("p (h d) -> p h d", h=4), op=ALU.mult),
                             reads=["r", "gain"], writes=[("gr", p)])

                    def b_stage2(c):
                        t0 = c * 64
                        tg = t0s + t0
                        p = c % 2
                        for h in range(4):
                            S.op("pe", lambda e, h=h: e.matmul(ps[4][0:64, 128 * h:128 * h + 128], att[:, p, h, :], v_sb[:, c, 128 * h:128 * h + 128], start=True, stop=False),
                                 reads=[("att", p), "v"], writes=[("ps", 4)])
                            S.op("pe", lambda e, h=h: e.matmul(ps[4][0:64, 128 * h:128 * h + 128], qd[:, p, h, :], Sb[:, h, :], start=False, stop=True),
                                 reads=[("qd", p), "Sb"], writes=[("ps", 4)])
                        for h in range(4):
                            S.op("pe", lambda e, h=h: e.matmul(ps[5][0:64, 128 * h:128 * h + 128], ktl[:, p, 64 * h:64 * h + 64], v_sb[:, c, 128 * h:128 * h + 128], start=True, stop=True),
                                 reads=[("ktl", p), "v"], writes=[("ps", 5)])
                        S.op("dve", lambda e: e.tensor_tensor(out=Sf[:], in0=Sf[:], in1=ecum[:, p, :, 63:64].to_broadcast([64, 4, 128]), op=ALU.mult), reads=["Sf", ("ecum", p)], writes=["Sf"])
                        S.op("dve", lambda e: e.tensor_tensor(out=Sf[:], in0=Sf[:], in1=ps[5][0:64, :].rearrange("p (h d) -> p h d", h=4), op=ALU.add), reads=["Sf", ("ps", 5)], writes=["Sf"])
                        S.op("act", lambda e: e.activation(out=Sb[:], in_=Sf[:], func=AF.Copy), reads=["Sf"], writes=["Sb"])
                        S.op("act", lambda e: e.activation(out=of[:], in_=ps[4][0:64, :].rearrange("p (h d) -> p h d", h=4), func=AF.Copy), reads=[("ps", 4)], writes=["of"])
                        head_norm(of[:], "of", 64, gr[:, p], ("gr", p), o_bf[:], "o_bf", sqt[:], ss[:], "B")
                        cc = (tg // 64) % 8
                        sg = (tg // 512) % 2
                        tb_ = ps[6][:, 0:128].bitcast(BF16)
                        for h in range(4):
                            S.op("pe", lambda e, h=h: e.transpose(tb_[:, 64 * h:64 * h + 64], o_bf[:, h, :], ident_b[0:64, 0:64]), reads=["o_bf", "ident_b"], writes=[("ps", 6)])
                        S.op("act", lambda e: e.activation(out=stg[:, sg, :, 64 * cc:64 * cc + 64], in_=tb_.rearrange("p (h t) -> p h t", h=4), func=AF.Copy),
                             reads=[("ps", 6)], writes=[("stg", sg)])
                        if cc == 7:
                            T0 = (tg // 512) * 512
                            S.dma("sp", lambda e: e.dma_start(out=brT[:, 4:8, T0:T0 + 512], in_=stg[:, sg]), reads=[("stg", sg)])

                    b_stage1(0)
                    for c in range(NCS):
                        if c + 1 < NCS:
                            b_stage1(c + 1)
                        b_stage2(c)
                S.emit()

        def mixer_C(l):
            with ExitStack() as _st:
                xc = _st.enter_context(nc.sbuf_tensor(U("xc"), [128, 2, 515], F32))
                acc = _st.enter_context(nc.sbuf_tensor(U("acc"), [128, 2, 512], F32))
                yb = _st.enter_context(nc.sbuf_tensor(U("yb"), [128, 2, 512], BF16))
                cw = _st.enter_context(nc.sbuf_tensor(U("cw"), [128, 8, 4], F32))
                cb = _st.enter_context(nc.sbuf_tensor(U("cb"), [128, 8], F32))
                S.dma("sp", lambda e: e.dma_start(out=cw[:], in_=dr["conv_w"][l]), writes=["cw"])
                S.dma("sp", lambda e: e.dma_start(out=cb[:], in_=dr["conv_b"][l]), writes=["cw"])
                n = 0
                for cc in range(8):
                    for tg in range(NT):
                        sl = n % 2
                        n += 1
                        t0 = tg * 512
                        if tg == 0:
                            S.op("pool", lambda e, sl=sl: e.memset(xc[:, sl, 0:3], 0.0), writes=[("xc", sl)])
                            S.dma("sp", lambda e, sl=sl, cc=cc: e.dma_start(out=xc[:, sl, 3:515], in_=cqk[cc][:, 0:512]), writes=[("xc", sl)])
                        else:
                            S.dma("sp", lambda e, sl=sl, cc=cc, t0=t0: e.dma_start(out=xc[:, sl, :], in_=cqk[cc][:, t0 - 3:t0 + 512]), writes=[("xc", sl)])
                        S.op("dve", lambda e, sl=sl, cc=cc: e.tensor_scalar(out=acc[:, sl, :], in0=xc[:, sl, 3:515], scalar1=cw[:, cc, 3:4], scalar2=cb[:, cc:cc + 1], op0=ALU.mult, op1=ALU.add),
                             reads=[("xc", sl), "cw"], writes=[("acc", sl)])
                        for j in range(3):
                            S.op("dve", lambda e, sl=sl, cc=cc, j=j: e.scalar_tensor_tensor(out=acc[:, sl, :], in0=xc[:, sl, j:j + 512], scalar=cw[:, cc, j:j + 1], in1=acc[:, sl, :],
                                                                                            op0=ALU.mult, op1=ALU.add), reads=[("xc", sl), "cw", ("acc", sl)], writes=[("acc", sl)])
                        if cc < 4:
                            S.op("act", lambda e, sl=sl: e.activation(out=yb[:, sl, :], in_=acc[:, sl, :], func=AF.Silu), reads=[("acc", sl)], writes=[("yb", sl)])
                        else:
                            S.op("act", lambda e, sl=sl: e.activation(out=acc[:, sl, :], in_=acc[:, sl, :], func=AF.Silu), reads=[("acc", sl)], writes=[("acc", sl)])
                            S.op("pool", lambda e, sl=sl: e.tensor_scalar(out=yb[:, sl, :], in0=acc[:, sl, :], scalar1=float(128 ** -0.5), scalar2=None, op0=ALU.mult),
                                 reads=[("acc", sl)], writes=[("yb", sl)])
                        S.dma("sp", lambda e, sl=sl, cc=cc, t0=t0: e.dma_start(out=cqk2[cc][:, t0:t0 + 512], in_=yb[:, sl, :]), reads=[("yb", sl)])
                S.emit()
            G = T // 128
            P4 = 4 * G
            TS = min(T, 1024)
            NCS = TS // 64
            with ExitStack() as st:
                def sb_(name, shape, dt):
                    return st.enter_context(nc.sbuf_tensor(U(name), shape, dt))
                ip = sb_("ip", [P4, 128], F32)
                fp = sb_("fp", [P4, 128], F32)
                nbc = sb_("nbc", [P4, 128], F32)
                gg = sb_("gg", [P4, 128], F32)
                gmx = sb_("gmx", [P4, 128], F32)
                mt_ = sb_("mt_", [P4, 128], F32)
                tq = sb_("tq", [P4, 128], F32)
                rt_ = sb_("rt_", [P4, 128], F32)
                wtl = sb_("wtl", [P4, 128], F32)
                enm = sb_("enm", [P4, 128], F32)
                rm = sb_("rm", [P4, 2, 128], F32)
                bif = sb_("bif", [P4, 3], F32)
                ge = sb_("ge", [P4, 2], F32)
                nbe = sb_("nbe", [P4, 2], F32)
                mp = sb_("mp", [P4, 2], F32)
                ch = sb_("ch", [4, 8, NCH], F32)
                sel = sb_("sel", [4, 4, 128], F32)
                abc = sb_("abc", [128, 4, 2, NCH], F32)
                mneg = sb_("mneg", [64, 64], F32)
                gain = sb_("gain", [64, 512], F32)
                onec = sb_("onec", [64, 2], BF16)
                tokv = lambda d_: d_.rearrange("h (g t) -> (h g) t", t=128)
                chv = lambda d_: d_.rearrange("h (g c) -> (h g) c", c=2)
                S.dma("sp", [lambda e: e.dma_start(out=ip[:], in_=tokv(cif[0:4, :])), lambda e: e.dma_start(out=fp[:], in_=tokv(cif[4:8, :]))], writes=["ipfp"])
                S.dma("sp", [lambda e: e.dma_start(out=bif[:, 0:1], in_=dr["b_i"][l]), lambda e: e.dma_start(out=bif[:, 1:2], in_=dr["b_f"][l])], writes=["bif"])
                S.dma("sp", [lambda e: e.dma_start(out=rm[:, 0, :], in_=dr["c_rmask01"][0:P4, :]), lambda e: e.dma_start(out=rm[:, 1, :], in_=dr["c_rmaskneg"][0:P4, :])], writes=["rm"])
                S.dma("sp", lambda e: e.dma_start(out=sel[:], in_=dr["c_sel"]), writes=["sel"])
                S.dma("sp", lambda e: e.dma_start(out=mneg[:], in_=dr["c_maskneg"]), writes=["mneg"])
                S.dma("sp", lambda e: e.dma_start(out=gain[:], in_=dr["mlstm_norm"][l, 0].partition_broadcast(64)), writes=["gain"])
                S.op("pool", lambda e: e.memset(onec[:], 1.0), writes=["onec"])
                S.op("dve", lambda e: e.tensor_scalar(out=ip[:], in0=ip[:], scalar1=bif[:, 0:1], scalar2=None, op0=ALU.add), reads=["ipfp", "bif"], writes=["li"])
                S.op("dve", lambda e: e.tensor_scalar(out=bif[:, 2:3], in0=bif[:, 1:2], scalar1=-1.0, scalar2=None, op0=ALU.mult), reads=["bif"], writes=["bif2"])
                S.op("act", lambda e: e.activation(out=fp[:], in_=fp[:], func=AF.Exp, scale=-1.0, bias=bif[:, 2:3]), reads=["ipfp", "bif2"], writes=["sp"])
                S.op("act", lambda e: e.activation(out=fp[:], in_=fp[:], func=AF.Ln, bias=1.0), reads=["sp"], writes=["sp"])
                S.op("dve", lambda e: e.tensor_tensor_scan(out=nbc[:], data0=rm[:, 0, :], data1=fp[:], initial=0.0, op0=ALU.mult, op1=ALU.add), reads=["sp", "rm"], writes=["nb"])
                S.op("dve", lambda e: e.tensor_tensor(out=gg[:], in0=ip[:], in1=nbc[:], op=ALU.add), reads=["li", "nb"], writes=["g"])
                S.op("dve", lambda e: e.tensor_tensor_scan(out=gmx[:], data0=rm[:, 1, :], data1=gg[:], initial=0.0, op0=ALU.add, op1=ALU.max), reads=["g", "rm"], writes=["gmx"])
                ends = lambda t_: t_[:, :].rearrange("p (c t) -> p c t", t=64)[:, :, 63]
                S.op("dve", lambda e: e.tensor_copy(out=nbe[:], in_=ends(nbc)), reads=["nb"], writes=["nbe"])
                S.op("dve", lambda e: e.tensor_copy(out=ge[:], in_=ends(gmx)), reads=["gmx"], writes=["ge"])
                S.dma("sp", [lambda e: e.dma_start(out=chv(chd[0]), in_=nbe[:]), lambda e: e.dma_start(out=chv(chd[1]), in_=ge[:])], reads=["nbe", "ge"], writes=["chd01"])
                S.dma("sp", [lambda e: e.dma_start(out=ch[:, 0, :], in_=chd[0]), lambda e: e.dma_start(out=ch[:, 1, :], in_=chd[1])], reads=["chd01"], writes=["ch0", "ch1"])
                S.op("dve", lambda e: e.tensor_tensor(out=ch[:, 2, :], in0=ch[:, 1, :], in1=ch[:, 0, :], op=ALU.subtract), reads=["ch0", "ch1"], writes=["ch2"])
                S.op("dve", lambda e: e.tensor_scalar(out=ch[:, 3, :], in0=ch[:, 0, :], scalar1=-1.0, scalar2=None, op0=ALU.mult), reads=["ch0"], writes=["ch3"])
                S.op("dve", lambda e: e.tensor_tensor_scan(out=ch[:, 4, :], data0=ch[:, 3, :], data1=ch[:, 2, :], initial=0.0, op0=ALU.add, op1=ALU.max), reads=["ch2", "ch3"], writes=["ch4"])
                S.op("dve", lambda e: e.memset(ch[:, 5, 0:1], 0.0), writes=["ch5a"])
                if NCH > 1:
                    S.op("dve", lambda e: e.tensor_copy(out=ch[:, 5, 1:NCH], in_=ch[:, 4, 0:NCH - 1]), reads=["ch4"], writes=["ch5b"])
                S.dma("sp", lambda e: e.dma_start(out=chd[2], in_=ch[:, 5, :]), reads=["ch5a", "ch5b"], writes=["chd2"])
                S.dma("sp", lambda e: e.dma_start(out=mp[:], in_=chv(chd[2])), reads=["chd2"], writes=["mp"])
                S.op("dve", lambda e: e.tensor_tensor(out=ch[:, 6, :], in0=ch[:, 3, :], in1=ch[:, 5, :], op=ALU.add), reads=["ch3", "ch5a", "ch5b"], writes=["ch6"])
                S.op("dve", lambda e: e.tensor_tensor(out=ch[:, 6, :], in0=ch[:, 6, :], in1=ch[:, 4, :], op=ALU.subtract), reads=["ch6", "ch4"], writes=["ch6"])
                S.op("dve", lambda e: e.tensor_tensor(out=ch[:, 7, :], in0=ch[:, 2, :], in1=ch[:, 4, :], op=ALU.subtract), reads=["ch2", "ch4"], writes=["ch7"])
                S.op("act", lambda e: e.activation(out=ch[:, 6:8, :], in_=ch[:, 6:8, :], func=AF.Exp), reads=["ch6", "ch7"], writes=["ch67"])
                v3 = lambda t_: t_[:, :].rearrange("p (c t) -> p c t", t=64)
                bc = lambda t_: t_[:, :].unsqueeze(2).to_broadcast([P4, 2, 64])
                S.op("dve", lambda e: e.tensor_tensor(out=v3(tq), in0=bc(mp), in1=v3(nbc), op=ALU.subtract), reads=["mp", "nb"], writes=["tq"])
                S.op("dve", lambda e: e.tensor_tensor(out=mt_[:], in0=gmx[:], in1=nbc[:], op=ALU.subtract), reads=["gmx", "nb"], writes=["mt"])
                S.op("dve", lambda e: e.tensor_tensor(out=mt_[:], in0=mt_[:], in1=tq[:], op=ALU.max), reads=["mt", "tq"], writes=["mt"])
                S.op("dve", lambda e: e.tensor_tensor(out=tq[:], in0=tq[:], in1=mt_[:], op=ALU.subtract), reads=["tq", "mt"], writes=["tq"])
                S.op("act", lambda e: e.activation(out=tq[:], in_=tq[:], func=AF.Exp), reads=["tq"], writes=["tq"])
                S.op("dve", lambda e: e.scalar_tensor_tensor(out=rt_[:], in0=nbc[:], scalar=-1.0, in1=mt_[:], op0=ALU.mult, op1=ALU.subtract), reads=["nb", "mt"], writes=["rt"])
                S.op("act", lambda e: e.activation(out=enm[:], in_=mt_[:], func=AF.Exp, scale=-1.0), reads=["mt"], writes=["enm"])
                S.op("dve", lambda e: e.tensor_tensor(out=v3(wtl), in0=v3(gg), in1=bc(ge), op=ALU.subtract), reads=["g", "ge"], writes=["wtl"])
                S.op("act", lambda e: e.activation(out=wtl[:], in_=wtl[:], func=AF.Exp), reads=["wtl"], writes=["wtl"])
                S.dma("sp", [lambda e: e.dma_start(out=tokv(stkd[0:4, :]), in_=gg[:]), lambda e: e.dma_start(out=tokv(stkd[4:8, :]), in_=wtl[:]),
                             lambda e: e.dma_start(out=tokv(stkd[8:12, :]), in_=tq[:]), lambda e: e.dma_start(out=tokv(stkd[12:16, :]), in_=enm[:]),
                             lambda e: e.dma_start(out=tokv(rtd), in_=rt_[:])], reads=["g", "wtl", "tq", "enm", "rt"], writes=["stkd"])
                for h in range(4):
                    S.op("pe", lambda e, h=h: e.matmul(ps[0][:, 0:2 * NCH], sel[:, h, :], ch[:, 6:8, :], start=True, stop=True), reads=["ch67", "sel"], writes=[("ps", 0)])
                    S.op("act", lambda e, h=h: e.activation(out=abc[:, h], in_=ps[0][:, 0:2 * NCH].rearrange("p (a c) -> p a c", a=2), func=AF.Copy), reads=[("ps", 0)], writes=["abc"])
                S.emit()
                stk = sb_("stk", [16, TS], F32)
                rt = sb_("rt", [4, TS], F32)
                q_sb = sb_("q_sb", [128, 4, TS], BF16)
                k_sb = sb_("k_sb", [128, 4, TS], BF16)
                v_sb = sb_("v_sb", [64, NCS, 512], BF16)
                og = sb_("og", [64, NCS, 512], F32)
                cols = sb_("cols", [64, 16], F32)
                kw = sb_("kw", [64, 4, 128], BF16)
                ex = sb_("ex", [64, 4, 64], F32)
                AT = sb_("AT", [64, 4, 64], BF16)
                Cf = sb_("Cf", [128, 4, 128], F32)
                Cb = sb_("Cb", [128, 4, 128], BF16)
                nf = sb_("nf", [128, 4], F32)
                nbb = sb_("nbb", [128, 4], BF16)
                t2 = sb_("t2", [64, 4, 128], F32)
                dn = sb_("dn", [64, 8], F32)
                tmpC = sb_("tmpC", [128, 4, 128], F32)
                tmpn = sb_("tmpn", [128, 4], F32)
                sqt = sb_("sqt", [64, 4, 128], F32)
                ss = sb_("ss", [64, 4], F32)
                o_bf = sb_("o_bf", [64, 4, 128], BF16)
                stg = sb_("stg", [128, 2, 4, 512], BF16)
                S.op("dve", lambda e: e.memset(Cf[:], 0.0), writes=["Cf"])
                S.op("pool", lambda e: e.memset(Cb[:], 0.0), writes=["Cb"])
                S.op("dve", lambda e: e.memset(nf[:], 0.0), writes=["nf"])
                S.op("pool", lambda e: e.memset(nbb[:], 0.0), writes=["nbb"])
                for ts in range(T // TS):
                    t0s = ts * TS
                    S.dma("sp", [lambda e, h=h, t0s=t0s: e.dma_start(out=q_sb[:, h, :], in_=cqk2[h][:, t0s:t0s + TS]) for h in range(4)] +
                          [lambda e, h=h, t0s=t0s: e.dma_start(out=k_sb[:, h, :], in_=cqk2[4 + h][:, t0s:t0s + TS]) for h in range(4)], writes=["qk"])
                    S.dma("sp", lambda e, t0s=t0s: e.dma_start(out=v_sb[:], in_=cv[t0s:t0s + TS, :].rearrange("(c p) n -> p c n", p=64)), writes=["v"])
                    S.dma("sp", lambda e, t0s=t0s: e.dma_start(out=og[:], in_=co[t0s:t0s + TS, :].rearrange("(c p) n -> p c n", p=64)), writes=["og"])
                    S.dma("sp", [lambda e, t0s=t0s: e.dma_start(out=stk[:], in_=stkd[:, t0s:t0s + TS]), lambda e, t0s=t0s: e.dma_start(out=rt[:], in_=rtd[:, t0s:t0s + TS])], writes=["stk", "rt"])
                    for c in range(NCS):
                        t0 = c * 64
                        tg = t0s + t0
                        cg = tg // 64
                        S.op("pe", lambda e, t0=t0: e.transpose(ps[0][0:64, 0:16], stk[:, t0:t0 + 64], ident_f[0:16, 0:16]),
                             reads=["stk", "ident_f"], writes=[("ps", 0)])
                        S.op("act", lambda e: e.activation(out=cols[:], in_=ps[0][0:64, 0:16], func=AF.Copy), reads=[("ps", 0)], writes=["cols"])
                        kb_ = ps[1][:, 0:256].bitcast(BF16)
                        for h in range(4):
                            S.op("pe", lambda e, h=h, t0=t0: e.transpose(kb_[0:64, 128 * h:128 * h + 128], k_sb[:, h, t0:t0 + 64], ident_b[:]), reads=["qk", "ident_b"], writes=[("ps", 1)])
                        S.op("dve", lambda e: e.tensor_tensor(out=kw[:], in0=kb_[0:64, :].rearrange("p (h d) -> p h d", h=4), in1=cols[:, 4:8].unsqueeze(2).to_broadcast([64, 4, 128]), op=ALU.mult),
                             reads=[("ps", 1), "cols"], writes=["kw"])
                        for h in range(4):
                            S.op("pe", lambda e, h=h, t0=t0: e.matmul(ps[2][0:64, 64 * h:64 * h + 64], k_sb[:, h, t0:t0 + 64], q_sb[:, h, t0:t0 + 64], start=True, stop=True),
                                 reads=["qk"], writes=[("ps", 2)])
                        for h in range(4):
                            S.op("pe", lambda e, h=h, t0=t0: e.matmul(ps[3][0:64, 64 * h:64 * h + 64], sel[:, h, 0:64], rt[:, t0:t0 + 64], start=True, stop=False),
                                 reads=["sel", "rt"], writes=[("ps", 3)])
                            S.op("pe", lambda e, h=h: e.matmul(ps[3][0:64, 64 * h:64 * h + 64], ident_f[0:64, 0:64], mneg[:], start=False, stop=True),
                                 reads=["ident_f", "mneg"], writes=[("ps", 3)])
                        for h in range(4):
                            S.op("act", lambda e, h=h: e.activation(out=ex[:, h, :], in_=ps[3][0:64, 64 * h:64 * h + 64], func=AF.Exp, bias=cols[:, h:h + 1]),
                                 reads=[("ps", 3), "cols"], writes=[("ex", h)])
                        S.op("dve", lambda e: e.tensor_tensor(out=AT[:], in0=ex[:], in1=ps[2][0:64, 0:256].rearrange("p (h t) -> p h t", h=4), op=ALU.mult),
                             reads=[("ex", 0), ("ex", 1), ("ex", 2), ("ex", 3), ("ps", 2)], writes=["AT"])
                        for h in range(4):
                            S.op("pe", lambda e, h=h, c=c: e.matmul(ps[4][0:64, 128 * h:128 * h + 128], AT[:, h, :], v_sb[:, c, 128 * h:128 * h + 128], start=True, stop=True),
                                 reads=["AT", "v"], writes=[("ps", 4)])
                            S.op("pe", lambda e, h=h: e.matmul(ps[7][0:64, 16 + h:17 + h], AT[:, h, :], onec[:, 0:1], start=True, stop=True), reads=["AT", "onec"], writes=[("ps7", "a")])
                        for h in range(4):
                            S.op("pe", lambda e, h=h, t0=t0: e.matmul(ps[5][0:64, 128 * h:128 * h + 128], q_sb[:, h, t0:t0 + 64], Cb[:, h, :], start=True, stop=True),
                                 reads=["qk", "Cb"], writes=[("ps", 5)])
                            S.op("pe", lambda e, h=h, t0=t0: e.matmul(ps[7][0:64, 20 + h:21 + h], q_sb[:, h, t0:t0 + 64], nbb[:, h:h + 1], start=True, stop=True),
                                 reads=["qk", "nbb"], writes=[("ps7", "b")])
                        S.op("dve", lambda e: e.tensor_tensor(out=t2[:], in0=ps[5][0:64, :].rearrange("p (h d) -> p h d", h=4), in1=cols[:, 8:12].unsqueeze(2).to_broadcast([64, 4, 128]), op=ALU.mult),
                             reads=[("ps", 5), "cols"], writes=["t2"])
                        S.op("dve", lambda e: e.tensor_tensor(out=t2[:], in0=t2[:], in1=ps[4][0:64, :].rearrange("p (h d) -> p h d", h=4), op=ALU.add), reads=["t2", ("ps", 4)], writes=["t2"])
                        S.op("dve", lambda e: e.tensor_tensor(out=dn[:, 0:4], in0=ps[7][0:64, 20:24], in1=cols[:, 8:12], op=ALU.mult), reads=[("ps7", "b"), "cols"], writes=["dn"])
                        S.op("dve", lambda e: e.tensor_tensor(out=dn[:, 0:4], in0=dn[:, 0:4], in1=ps[7][0:64, 16:20], op=ALU.add), reads=["dn", ("ps7", "a")], writes=["dn"])
                        S.op("act", lambda e: e.activation(out=dn[:, 0:4], in_=dn[:, 0:4], func=AF.Abs), reads=["dn"], writes=["dn"])
                        S.op("dve", lambda e: e.tensor_tensor(out=dn[:, 0:4], in0=dn[:, 0:4], in1=cols[:, 12:16], op=ALU.max), reads=["dn", "cols"], writes=["dn"])
                        S.op("dve", lambda e: e.reciprocal(out=dn[:, 0:4], in_=dn[:, 0:4]), reads=["dn"], writes=["dn"])
                        S.op("dve", lambda e: e.tensor_tensor(out=t2[:], in0=t2[:], in1=dn[:, 0:4].unsqueeze(2).to_broadcast([64, 4, 128]), op=ALU.mult), reads=["t2", "dn"], writes=["t2"])
                        S.op("dve", lambda e, c=c: e.tensor_tensor(out=t2[:], in0=t2[:], in1=og[:, c, :].rearrange("p (h d) -> p h d", h=4), op=ALU.mult), reads=["t2", "og"], writes=["t2"])
                        head_norm(t2[:], "t2", 64, gain[:, :].rearrange("p (h d) -> p h d", h=4), "gain", o_bf[:], "o_bf", sqt[:], ss[:], "C")
                        cc = cg % 8
                        sg = (tg // 512) % 2
                        tb_ = ps[6][:, 0:128].bitcast(BF16)
                        for h in range(4):
                            S.op("pe", lambda e, h=h: e.transpose(tb_[:, 64 * h:64 * h + 64], o_bf[:, h, :], ident_b[0:64, 0:64]), reads=["o_bf", "ident_b"], writes=[("ps", 6)])
                        S.op("act", lambda e, sg=sg, cc=cc: e.activation(out=stg[:, sg, :, 64 * cc:64 * cc + 64], in_=tb_.rearrange("p (h t) -> p h t", h=4), func=AF.Copy),
                             reads=[("ps", 6)], writes=[("stg", sg)])
                        if cc == 7:
                            T0 = (tg // 512) * 512
                            S.dma("sp", lambda e, sg=sg, T0=T0: e.dma_start(out=brT[:, 8:12, T0:T0 + 512], in_=stg[:, sg]), reads=[("stg", sg)])
                        for h in range(4):
                            S.op("pe", lambda e, h=h, c=c: e.matmul(ps[1][:, 128 * h:128 * h + 128], kw[:, h, :], v_sb[:, c, 128 * h:128 * h + 128], start=True, stop=True),
                                 reads=["kw", "v"], writes=[("ps", 1)])
                            S.op("pe", lambda e, h=h: e.matmul(ps[7][:, 24 + h:25 + h], kw[:, h, :], onec[:, 0:1], start=True, stop=True), reads=["kw", "onec"], writes=[("ps7", "c")])
                        a_bc = lambda cg=cg: abc[:, :, 0, cg:cg + 1].to_broadcast([128, 4, 128])
                        b_bc = lambda cg=cg: abc[:, :, 1, cg:cg + 1].to_broadcast([128, 4, 128])
                        S.op("dve", lambda e, cg=cg: e.tensor_tensor(out=tmpC[:], in0=ps[1][:, :].rearrange("p (h d) -> p h d", h=4), in1=abc[:, :, 1, cg:cg + 1].to_broadcast([128, 4, 128]), op=ALU.mult),
                             reads=[("ps", 1), "abc"], writes=["tmpC"])
                        S.op("pool", lambda e, cg=cg: e.tensor_tensor(out=Cf[:], in0=Cf[:], in1=abc[:, :, 0, cg:cg + 1].to_broadcast([128, 4, 128]), op=ALU.mult), reads=["Cf", "abc"], writes=["Cf"])
                        S.op("pool", lambda e: e.tensor_tensor(out=Cf[:], in0=Cf[:], in1=tmpC[:], op=ALU.add), reads=["Cf", "tmpC"], writes=["Cf"])
                        S.op("act", lambda e: e.activation(out=Cb[:], in_=Cf[:], func=AF.Copy), reads=["Cf"], writes=["Cb"])
                        S.op("dve", lambda e, cg=cg: e.tensor_tensor(out=tmpn[:], in0=ps[7][:, 24:28], in1=abc[:, :, 1, cg], op=ALU.mult), reads=[("ps7", "c"), "abc"], writes=["tmpn"])
                        S.op("dve", lambda e, cg=cg: e.tensor_tensor(out=nf[:], in0=nf[:], in1=abc[:, :, 0, cg], op=ALU.mult), reads=["nf", "abc"], writes=["nf"])
                        S.op("dve", lambda e: e.tensor_tensor(out=nf[:], in0=nf[:], in1=tmpn[:], op=ALU.add), reads=["nf", "tmpn"], writes=["nf"])
                        S.op("dve", lambda e: e.tensor_copy(out=nbb[:], in_=nf[:]), reads=["nf"], writes=["nbb"])
                S.emit()

        stages = dbg if isinstance(dbg, dict) else {}
        upto = stages.get("upto", "all")
        phase_in()
        for l in range(L):
            phase_inproj(l)
            if upto == "inproj":
                break
            phase_gates(l)
            if "skipA" not in stages:
                mixer_A(l)
            if "skipB" not in stages:
                mixer_B(l)
            if "skipC" not in stages:
                mixer_C(l)
            if "skipD" not in stages:
                mixer_D(l)
            if upto == "mix":
                break
            phase_merge(l)
            if upto == "merge":
                break
            phase_outproj(l)
            if upto == "outproj":
                break
            phase_norm(dr["g_mix_post"][l], dr["g_ffn_pre"][l], False)
            if upto == "norm1":
                break
            phase_ffn_up(l)
            if upto == "up":
                break
            phase_ffn_down(l)
            if upto == "down":
                break
            last = (l == L - 1)
            phase_norm(dr["g_ffn_post"][l], None if last else dr["g_mix_pre"][l + 1], last)
    return nc


_CACHE = {}


def kernel(**inputs):
    T, L, NCORE = 4096, 4, 8
    x = np.asarray(inputs["x"], dtype=np.float32)
    if "nc" not in _CACHE:
        _CACHE["nc"] = build(T, L)
    nc = _CACHE["nc"]
    params = prep_params(inputs, L, T)
    cs = make_consts(T)
    shared = {k: np.ascontiguousarray(np.asarray(inputs[k], dtype=np.float32)) for k in BIGW(L)}
    shared.update(params)
    shared.update({"c_" + k: np.ascontiguousarray(v) for k, v in cs.items()})
    in_maps = []
    for b in range(NCORE):
        m = dict(shared)
        m["x"] = np.ascontiguousarray(x[b])
        in_maps.append(m)
    res = run_bass_kernel_spmd(nc, in_maps, core_ids=list(range(NCORE)))
    return np.stack([np.asarray(r["out"], dtype=np.float32) for r in res.results], axis=0)
```

```python
import math
import numpy as np
import ml_dtypes
from contextlib import ExitStack
import concourse.bass as bass
import concourse.mybir as mybir
from concourse.bass_utils import run_bass_kernel_spmd

F32 = mybir.dt.float32
BF16 = mybir.dt.bfloat16
AF = mybir.ActivationFunctionType
ALU = mybir.AluOpType
AX = mybir.AxisListType

D = 2048
DC = 16
DFF = 8192
NIN = 6680
EPS = 1e-6
A0, B0, C0, D0 = 0, 1536, 3088, 5144
ENGS = ("pe", "act", "dve", "pool", "sp")


class Sched:
    def __init__(self, nc, es, n_dma_sems=10):
        self.nc = nc
        self.sem = {e: es.enter_context(nc.semaphore("s_" + e)) for e in ENGS}
        self.cnt = {e: 0 for e in ENGS}
        self.dq = ("sp", "pool")
        self.dsems = {q: [es.enter_context(nc.semaphore(f"d_{q}{i}")) for i in range(n_dma_sems)] for q in self.dq}
        self.dcnt = {q: [0] * n_dma_sems for q in self.dq}
        self.dnext = {q: 0 for q in self.dq}
        self.seen = {e: {} for e in ENGS}
        self.bar = es.enter_context(nc.semaphore("s_bar"))
        self.barcnt = 0
        self.psn = 0
        self.reset()

    def reset(self):
        self.ops = {e: [] for e in ENGS}
        self.lastw = {}
        self.readers = {}

    def _add(self, eng, rec, reads, writes):
        idx = len(self.ops[eng])
        deps = set()
        for k in list(reads) + list(writes):
            lw = self.lastw.get(k)
            if lw is not None:
                deps.add(lw)
        for k in writes:
            for r in self.readers.get(k, ()):
                deps.add(r)
        deps.discard((eng, idx))
        rec["deps"] = deps
        rec["signal"] = bool(rec["dma"])
        self.ops[eng].append(rec)
        for k in writes:
            self.lastw[k] = (eng, idx)
            self.readers[k] = []
        for k in reads:
            self.readers.setdefault(k, []).append((eng, idx))

    def op(self, eng, fn, reads=(), writes=()):
        self._add(eng, {"fn": fn, "dma": False}, reads, writes)

    def dma(self, q, fns, reads=(), writes=()):
        if not isinstance(fns, (list, tuple)):
            fns = [fns]
        self._add(q, {"fn": list(fns), "dma": True}, reads, writes)

    def emit(self):
        nc = self.nc
        ops = self.ops
        for e in ENGS:
            for rec in ops[e]:
                for (de, di) in rec["deps"]:
                    d = ops[de][di]
                    if de == e and not d["dma"] and e == "pe":
                        continue
                    d["signal"] = True
        for e in ENGS:
            comp = [r for r in ops[e] if not r["dma"]]
            if comp:
                comp[-1]["signal"] = True
        for e in ENGS:
            for rec in ops[e]:
                if not rec["signal"]:
                    continue
                if rec["dma"]:
                    j = self.dnext[e]
                    self.dnext[e] = (j + 1) % len(self.dsems[e])
                    prev = self.dcnt[e][j]
                    self.dcnt[e][j] = prev + 16 * len(rec["fn"])
                    rec["sem"] = self.dsems[e][j]
                    rec["val"] = self.dcnt[e][j]
                    rec["prev"] = prev
                else:
                    self.cnt[e] += 1
                    rec["sem"] = self.sem[e]
                    rec["val"] = self.cnt[e]
        self.barcnt += 1
        barval = self.barcnt * len(ENGS)
        with nc.Block() as block:
            def body(e):
                def f(eng):
                    seen = self.seen[e]

                    def wait(sem, val):
                        key = id(sem)
                        if seen.get(key, 0) >= val:
                            return
                        eng.wait_ge(sem, val)
                        seen[key] = val
                    for rec in ops[e]:
                        for (de, di) in sorted(rec["deps"]):
                            d = ops[de][di]
                            if not d["signal"]:
                                continue
                            if de == e and not d["dma"] and e == "pe":
                                continue
                            wait(d["sem"], d["val"])
                        if rec["dma"]:
                            if rec["prev"] > 0:
                                wait(rec["sem"], rec["prev"])
                            for fn in rec["fn"]:
                                fn(eng).then_inc(rec["sem"], 16)
                        else:
                            ins = rec["fn"](eng)
                            if rec["signal"]:
                                ins.then_inc(rec["sem"], 1)
                    if e in self.dsems:
                        for j, s in enumerate(self.dsems[e]):
                            if self.dcnt[e][j] > 0:
                                wait(s, self.dcnt[e][j])
                    if self.cnt[e] > 0:
                        wait(self.sem[e], self.cnt[e])
                    eng.sem_inc(self.bar, 1)
                    eng.wait_ge(self.bar, barval)
                return f
            block.tensor(body("pe"))
            block.scalar(body("act"))
            block.vector(body("dve"))
            block.gpsimd(body("pool"))
            block.sync(body("sp"))
        self.reset()


def make_consts(T):
    bf = ml_dtypes.bfloat16
    c = {}
    c["ident_f"] = np.eye(128, dtype=np.float32)
    c["ident_b"] = np.eye(128, dtype=np.float32).astype(bf)
    c["ones_b"] = np.ones((128, 128), np.float32).astype(bf)
    slopes = [2.0 ** (-8.0 * (h + 1) / 4) for h in range(4)]
    t = np.arange(T)
    Aq, bq = (t // 128).astype(np.float64), (t % 128).astype(np.float64)
    augq = np.zeros((4, 4, T), np.float32)
    augk = np.zeros((4, 4, T), np.float32)
    for h, s in enumerate(slopes):
        augq[h, 0] = -s * 128 * Aq
        augq[h, 1] = -s * bq
        augq[h, 2] = 1
        augq[h, 3] = 1
        augk[h, 0] = 1
        augk[h, 1] = 1
        augk[h, 2] = s * 128 * Aq
        augk[h, 3] = s * bq
    c["augq"] = augq.astype(bf)
    c["augk"] = augk.astype(bf)
    kk = np.arange(128)[:, None]
    qq = np.arange(128)[None, :]
    cd = np.zeros((128, 4, 128), np.float32)
    for h, s in enumerate(slopes):
        cd[:, h, :] = -2 * s * np.maximum(kk - qq, 0) - 30000.0 * ((kk // 64) > (qq // 64))
    c["cdiag"] = cd
    m0 = np.zeros((128, 128), np.float32)
    m0[64:, :64] = -30000.0
    m4 = np.zeros((128, 128), np.float32)
    m4[:64, 64:] = -30000.0
    c["dmask"] = np.stack([m0, m4], axis=1)
    s64 = np.arange(64)[:, None]
    t64 = np.arange(64)[None, :]
    c["triu"] = np.where(s64 <= t64, -1.0 / 16, 0.0).astype(np.float32)
    c["trisl"] = np.where(s64 > t64, -1.0 / 16, 0.0).astype(np.float32)
    c["mask01"] = np.where(s64 <= t64, 1.0, 0.0).astype(np.float32)
    c["maskneg"] = np.where(s64 > t64, -30000.0, 0.0).astype(np.float32)
    sel = np.zeros((4, 4, 128), np.float32)
    for h in range(4):
        sel[h, h, :] = 1
    c["sel"] = sel
    t128 = np.arange(128)
    c["rmask01"] = np.tile(np.where(t128 % 64 == 0, 0.0, 1.0).astype(np.float32), (128, 1))
    c["rmaskneg"] = np.tile(np.where(t128 % 64 == 0, -1e30, 0.0).astype(np.float32), (128, 1))
    c["onesrow"] = np.ones((1, T), np.float32)
    return c


CONST_SPECS = {"ident_f": F32, "ident_b": BF16, "ones_b": BF16, "augq": BF16, "augk": BF16, "cdiag": F32,
               "dmask": F32, "triu": F32, "trisl": F32, "mask01": F32, "maskneg": F32, "sel": F32,
               "rmask01": F32, "rmaskneg": F32, "onesrow": F32}


def prep_params(inp, L, T):
    f = lambda a: np.ascontiguousarray(np.asarray(a, dtype=np.float32))
    p = {}
    colmaj = lambda a: f(np.asarray(a).reshape(L, DC, 128).transpose(0, 2, 1))
    p["g_mix_pre"] = colmaj(inp["norm_mix_pre"])
    p["g_mix_post"] = colmaj(inp["norm_mix_post"])
    p["g_ffn_pre"] = colmaj(inp["norm_ffn_pre"])
    p["g_ffn_post"] = colmaj(inp["norm_ffn_post"])
    p["b_gate_c"] = f(np.asarray(inp["b_gate"]).reshape(L, 4, DC, 128).transpose(0, 3, 1, 2))
    p["diff_lambda"] = f(np.asarray(inp["diff_lambda"]).reshape(L, 1, 256))
    p["diff_norm"] = f(np.asarray(inp["diff_norm"]).reshape(L, 1, 512))
    p["gla_norm"] = f(np.asarray(inp["gla_norm"]).reshape(L, 1, 512))
    p["mlstm_norm"] = f(np.asarray(inp["mlstm_norm"]).reshape(L, 1, 512))
    wg = np.concatenate([np.asarray(inp["gla_w_gate_up"]), np.asarray(inp["gla_b_gate"])[:, None, :]], axis=1)
    p["gla_wg"] = f(wg)
    p["conv_w"] = f(np.asarray(inp["mlstm_conv_w"]).reshape(L, 4, 8, 128).transpose(0, 3, 2, 1))
    p["conv_b"] = f(np.asarray(inp["mlstm_conv_b"]).reshape(L, 8, 128).transpose(0, 2, 1))
    p["b_i"] = f(np.repeat(np.asarray(inp["mlstm_b_i"]).reshape(L, 4), T // 128, axis=1).reshape(L, 4 * (T // 128), 1))
    p["b_f"] = f(np.repeat(np.asarray(inp["mlstm_b_f"]).reshape(L, 4), T // 128, axis=1).reshape(L, 4 * (T // 128), 1))
    rb = np.asarray(inp["rel_bias"], dtype=np.float32)
    kk = np.arange(128)[:, None]
    qq = np.arange(128)[None, :]
    tiles = []
    for dl in (0, 1):
        idx = np.clip(128 * dl + qq - kk, -128, 128) + 128
        tiles.append(rb[:, :, idx])
    p["d_bias"] = f(np.stack(tiles, axis=2).transpose(0, 3, 2, 1, 4))
    p["d_far"] = f(np.broadcast_to(rb[:, None, :, 256], (L, 128, 4)))
    return p


PARAM_SHAPES = lambda L, T: {
    "g_mix_pre": [L, 128, 16], "g_mix_post": [L, 128, 16], "g_ffn_pre": [L, 128, 16], "g_ffn_post": [L, 128, 16],
    "b_gate_c": [L, 128, 4, 16], "diff_lambda": [L, 1, 256], "diff_norm": [L, 1, 512], "gla_norm": [L, 1, 512],
    "mlstm_norm": [L, 1, 512], "gla_wg": [L, 17, 256], "conv_w": [L, 128, 8, 4], "conv_b": [L, 128, 8],
    "b_i": [L, 4 * (T // 128), 1], "b_f": [L, 4 * (T // 128), 1], "d_bias": [L, 128, 2, 4, 128], "d_far": [L, 128, 4]}

BIGW = lambda L: {"w_in": [L, D, NIN], "w_branch": [L, 4, 512, D], "w_gate": [L, 4, D, D], "w_out": [L, D, D],
                  "w_up": [L, D, DFF], "w_down": [L, DFF, D]}


def build(T, L, dbg=()):
    nc = bass.Bass("TRN2", target_bir_lowering=False)
    NT = T // 512
    NB = T // 128
    NCH = T // 64
    dr = {}
    _uid = [0]

    def U(n):
        _uid[0] += 1
        return f"{n}_{_uid[0]}"

    def din(name, shape, dt):
        dr[name] = nc.dram_tensor(name, list(shape), dt, kind="ExternalInput").ap()
        return dr[name]

    def scr(name, shape, dt):
        kind = "ExternalOutput" if name in dbg else "Internal"
        dr[name] = nc.dram_tensor(name, list(shape), dt, kind=kind).ap()
        return dr[name]

    x_in = din("x", [T, D], F32)
    for k, shp in BIGW(L).items():
        din(k, shp, F32)
    for k, shp in PARAM_SHAPES(L, T).items():
        din(k, shp, F32)
    cs = make_consts(T)
    for k, dt in CONST_SPECS.items():
        din("c_" + k, cs[k].shape, dt)
    out = nc.dram_tensor("out", [T, D], F32, kind="ExternalOutput").ap()

    xT = scr("xT", [128, DC, T], F32)
    yT = scr("yT", [128, DC, T], F32)
    hT = scr("hT", [128, DC, T], BF16)
    mT = scr("mT", [128, DC, T], BF16)
    brT = scr("brT", [128, DC, T], BF16)
    fT = scr("fT", [128, 64, T], BF16)
    gT = scr("gT", [4, 128, DC, T], BF16)
    aq = scr("aq", [8, 64, T], BF16)
    ak = scr("ak", [8, 64, T], BF16)
    av = scr("av", [T, 512], BF16)
    bq = scr("bq", [4, 64, T], F32)
    bk = scr("bk", [4, 64, T], F32)
    bkT = scr("bkT", [T, 256], F32)
    bv = scr("bv", [T, 512], BF16)
    bg = scr("bg", [16, T], F32)
    br = scr("br", [T, 512], F32)
    cqk = scr("cqk", [8, 128, T], F32)
    cqk2 = scr("cqk2", [8, 128, T], BF16)
    cv = scr("cv", [T, 512], BF16)
    co = scr("co", [T, 512], F32)
    cif = scr("cif", [8, T], F32)
    dq = scr("dq", [4, 128, T], BF16)
    dk = scr("dk", [4, 128, T], BF16)
    dv = scr("dv", [T, 512], BF16)
    chd = scr("chd", [3, 4, NCH], F32)
    stkd = scr("stkd", [16, T], F32)
    rtd = scr("rtd", [4, T], F32)

    with ExitStack() as es:
        S = Sched(nc, es)
        ps = [es.enter_context(nc.psum_tensor(f"ps{i}", [128, 512], F32)) for i in range(8)]
        ident_f = es.enter_context(nc.sbuf_tensor(U("ident_f"), [128, 128], F32))
        ident_b = es.enter_context(nc.sbuf_tensor(U("ident_b"), [128, 128], BF16))
        ones_b = es.enter_context(nc.sbuf_tensor(U("ones_b"), [128, 128], BF16))
        S.dma("sp", lambda e: e.dma_start(out=ident_f[:], in_=dr["c_ident_f"]), writes=["ident_f"])
        S.dma("sp", lambda e: e.dma_start(out=ident_b[:], in_=dr["c_ident_b"]), writes=["ident_b"])
        S.dma("sp", lambda e: e.dma_start(out=ones_b[:], in_=dr["c_ones_b"]), writes=["ones_b"])
        S.emit()

        cnt = {"ev": 0}

        def evac_eng():
            cnt["ev"] += 1
            return "act" if cnt["ev"] % 2 == 0 else "dve"

        def next_bank(nb=6):
            i = S.psn % nb
            S.psn += 1
            return i

        def norm_tile(xt, xkey, gcol, tgn, h_out, hkey, sq, rs, psb):
            for c4 in range(4):
                S.op("act", lambda e, c4=c4: e.activation(out=sq[:, 4 * c4:4 * c4 + 4, :], in_=xt[:, 4 * c4:4 * c4 + 4, :], func=AF.Square),
                     reads=[xkey], writes=[("sq", c4)])
            for c in range(DC):
                S.op("pe", lambda e, c=c: e.matmul(ps[psb][:, 0:tgn], ones_b[:], sq[:, c, :], start=(c == 0), stop=(c == DC - 1)),
                     reads=[("sq", c // 4), "ones_b"], writes=[("ps", psb)])
            S.op("act", lambda e: e.activation(out=rs[:, 0:tgn], in_=ps[psb][:, 0:tgn], func=AF.Sqrt, bias=EPS, scale=1.0 / D),
                 reads=[("ps", psb)], writes=["rs"])
            S.op("dve", lambda e: e.reciprocal(out=rs[:, 0:tgn], in_=rs[:, 0:tgn]), reads=["rs"], writes=["rs"])
            for c in range(DC):
                S.op("dve", lambda e, c=c: e.scalar_tensor_tensor(out=h_out[:, c, :], in0=xt[:, c, :], scalar=gcol[:, c:c + 1], in1=rs[:, 0:tgn],
                                                                   op0=ALU.mult, op1=ALU.mult),
                     reads=[xkey, "rs", "gcol"], writes=[hkey])

        def phase_in():
            with ExitStack() as _st:
                xin = _st.enter_context(nc.sbuf_tensor(U("xin"), [128, 2, D], F32))
                xt = _st.enter_context(nc.sbuf_tensor(U("xt"), [128, DC, 512], F32))
                sq = _st.enter_context(nc.sbuf_tensor(U("sq"), [128, DC, 512], BF16))
                rs = _st.enter_context(nc.sbuf_tensor(U("rs"), [128, 512], F32))
                ht = _st.enter_context(nc.sbuf_tensor(U("ht"), [128, DC, 512], BF16))
                gcol = _st.enter_context(nc.sbuf_tensor(U("gcol"), [128, DC], F32))
                S.dma("sp", lambda e: e.dma_start(out=gcol[:], in_=dr["g_mix_pre"][0]), writes=["gcol"])
                for tg in range(NT):
                    for tb in range(4):
                        r0 = tg * 512 + tb * 128
                        sl = (tg * 4 + tb) % 2
                        S.dma("sp", lambda e, r0=r0, sl=sl: e.dma_start(out=xin[:, sl, :], in_=x_in[r0:r0 + 128, :]), writes=[("xin", sl)])
                        for cg in range(4):
                            b = next_bank()
                            for j in range(4):
                                c = cg * 4 + j
                                S.op("pe", lambda e, b=b, j=j, c=c, sl=sl: e.transpose(ps[b][:, 128 * j:128 * j + 128], xin[:, sl, 128 * c:128 * c + 128], ident_f[:]),
                                     reads=[("xin", sl), "ident_f"], writes=[("ps", b)])
                            if evac_eng() == "act":
                                S.op("act", lambda e, b=b, cg=cg, tb=tb: e.activation(out=xt[:, 4 * cg:4 * cg + 4, 128 * tb:128 * tb + 128],
                                                                                     in_=ps[b][:, :].rearrange("p (j t) -> p j t", j=4), func=AF.Copy),
                                     reads=[("ps", b)], writes=["xt"])
                            else:
                                S.op("dve", lambda e, b=b, cg=cg, tb=tb: e.tensor_copy(out=xt[:, 4 * cg:4 * cg + 4, 128 * tb:128 * tb + 128],
                                                                                      in_=ps[b][:, :].rearrange("p (j t) -> p j t", j=4)),
                                     reads=[("ps", b)], writes=["xt"])
                    S.dma("sp", lambda e, tg=tg: e.dma_start(out=xT[:, :, tg * 512:(tg + 1) * 512], in_=xt[:]), reads=["xt"])
                    norm_tile(xt, "xt", gcol, 512, ht, "ht", sq, rs, 7)
                    S.dma("sp", lambda e, tg=tg: e.dma_start(out=hT[:, :, tg * 512:(tg + 1) * 512], in_=ht[:]), reads=["ht"])
                S.emit()

        def gemm(A_dram, KC, TS, wblocks, wb_cols, stf_n=4):
            with ExitStack() as _st:
                A_sb = _st.enter_context(nc.sbuf_tensor(U("A_sb"), [128, KC, TS], BF16))
                wt = _st.enter_context(nc.sbuf_tensor(U("wt"), [128, 2, KC, wb_cols], BF16))
                stF = _st.enter_context(nc.sbuf_tensor(U("stF"), [128, stf_n, 512], F32))
                stB = _st.enter_context(nc.sbuf_tensor(U("stB"), [128, stf_n, 512], BF16))
                stc = [0]

                def loadw(i):
                    w_ap, ncols, _ = wblocks[i]
                    sl = i % 2
                    fns = []
                    step = max(1, KC // 4)
                    for k0 in range(0, KC, step):
                        fns.append(lambda e, k0=k0, sl=sl, w_ap=w_ap, ncols=ncols, step=step: e.dma_start(
                            out=wt[:, sl, k0:k0 + step, 0:ncols],
                            in_=w_ap[k0 * 128:(k0 + step) * 128, :].rearrange("(c p) n -> p c n", p=128)))
                    S.dma("pool", fns, writes=[("w", sl)])
                for ts in range(T // TS):
                    for k in range(KC):
                        S.dma("sp", lambda e, k=k, ts=ts: e.dma_start(out=A_sb[:, k, :], in_=A_dram[:, k, ts * TS:(ts + 1) * TS]), writes=[("A", k)])
                    loadw(0)
                    for i, (w_ap, ncols, jobs) in enumerate(wblocks):
                        if i + 1 < len(wblocks):
                            loadw(i + 1)
                        sl = i % 2
                        for (kind, c0, m, epi) in jobs:
                            if kind == "fm":
                                for tg in range(TS // 512):
                                    b = next_bank()
                                    for k in range(KC):
                                        S.op("pe", lambda e, b=b, k=k, sl=sl, c0=c0, m=m, tg=tg: e.matmul(
                                            ps[b][0:m, 0:512], wt[:, sl, k, c0:c0 + m], A_sb[:, k, tg * 512:(tg + 1) * 512],
                                            start=(k == 0), stop=(k == KC - 1)), reads=[("A", k), ("w", sl)], writes=[("ps", b)])
                                    s_ = stc[0] % stf_n
                                    stc[0] += 1
                                    epi(ps[b][0:m, 0:512], ts * TS + tg * 512, ("ps", b), stF[0:m, s_, :], stB[0:m, s_, :], ("st", s_))
                            else:
                                for tb in range(TS // 128):
                                    b = next_bank()
                                    for k in range(KC):
                                        S.op("pe", lambda e, b=b, k=k, sl=sl, c0=c0, m=m, tb=tb: e.matmul(
                                            ps[b][:, 0:m], A_sb[:, k, tb * 128:(tb + 1) * 128], wt[:, sl, k, c0:c0 + m],
                                            start=(k == 0), stop=(k == KC - 1)), reads=[("A", k), ("w", sl)], writes=[("ps", b)])
                                    s_ = stc[0] % stf_n
                                    stc[0] += 1
                                    epi(ps[b][:, 0:m], ts * TS + tb * 128, ("ps", b), stF[:, s_, 0:m], stB[:, s_, 0:m], ("st", s_))
                S.emit()

        def epi_simple(dst_fn, dt, func=None, scale=None, bias=None, post=None):
            def epi(psap, t0, pskey, stF, stB, skey):
                st = stB if dt == BF16 else stF
                reads = [pskey]
                if bias is not None:
                    reads.append("bias")
                if post == "relu2":
                    S.op("act", lambda e: e.activation(out=stF, in_=psap, func=AF.Relu), reads=reads, writes=[skey])
                    eng = "pool" if (t0 // 512) % 2 == 0 else "dve"
                    S.op(eng, lambda e: e.tensor_tensor(out=stB, in0=stF, in1=stF, op=ALU.mult), reads=[skey], writes=[skey])
                    st = stB
                elif func is not None or bias is not None:
                    kw = {}
                    if bias is not None:
                        kw["bias"] = bias
                    if scale is not None:
                        kw["scale"] = scale
                    S.op("act", lambda e: e.activation(out=st, in_=psap, func=(func or AF.Identity), **kw), reads=reads, writes=[skey])
                else:
                    eng = evac_eng()
                    if eng == "act":
                        S.op("act", lambda e: e.activation(out=st, in_=psap, func=AF.Copy, scale=(1.0 if scale is None else scale)), reads=reads, writes=[skey])
                    elif scale is None:
                        S.op("dve", lambda e: e.tensor_copy(out=st, in_=psap), reads=reads, writes=[skey])
                    else:
                        S.op("dve", lambda e: e.tensor_scalar(out=st, in0=psap, scalar1=float(scale), scalar2=None, op0=ALU.mult), reads=reads, writes=[skey])
                S.dma("sp", lambda e: e.dma_start(out=dst_fn(t0), in_=st), reads=[skey])
            return epi

        def phase_inproj(l):
            w = dr["w_in"][l]
            blocks = []

            def blk(c0, n, jobs):
                blocks.append((w[:, c0:c0 + n], n, jobs))
            fm = lambda dst, m, **kw: epi_simple(lambda t0: dst[:, t0:t0 + 512], **kw)
            tm = lambda dst, n, **kw: epi_simple(lambda t0: dst[t0:t0 + 128, 0:n], **kw)
            aqf = aq.rearrange("h d t -> (h d) t")
            akf = ak.rearrange("h d t -> (h d) t")
            bqf = bq.rearrange("h d t -> (h d) t")
            bkf = bk.rearrange("h d t -> (h d) t")
            blk(A0, 512, [("fm", 128 * j, 128, fm(aqf[128 * j:128 * j + 128], 128, dt=BF16, scale=0.125)) for j in range(4)])
            blk(A0 + 512, 512, [("fm", 128 * j, 128, fm(akf[128 * j:128 * j + 128], 128, dt=BF16)) for j in range(4)])
            blk(A0 + 1024, 512, [("tm", 0, 512, tm(av, 512, dt=BF16))])
            blk(B0, 512, [("fm", 128 * j, 128, fm(bqf[128 * j:128 * j + 128], 128, dt=F32, scale=0.125)) for j in range(2)] +
                [("fm", 256 + 128 * j, 128, fm(bkf[128 * j:128 * j + 128], 128, dt=F32)) for j in range(2)] +
                [("tm", 256, 256, tm(bkT, 256, dt=F32))])
            blk(B0 + 512, 512, [("tm", 0, 512, tm(bv, 512, dt=BF16))])
            blk(B0 + 1024, 16, [("fm", 0, 16, fm(bg, 16, dt=F32))])
            blk(B0 + 1040, 512, [("tm", 0, 512, tm(br, 512, dt=F32, func=AF.Silu))])
            blk(C0, 512, [("fm", 128 * j, 128, fm(cqk[j], 128, dt=F32)) for j in range(4)])
            blk(C0 + 512, 512, [("fm", 128 * j, 128, fm(cqk[4 + j], 128, dt=F32)) for j in range(4)])
            blk(C0 + 1024, 512, [("tm", 0, 512, tm(cv, 512, dt=BF16))])
            blk(C0 + 1536, 512, [("tm", 0, 512, tm(co, 512, dt=F32, func=AF.Sigmoid))])
            blk(C0 + 2048, 8, [("fm", 0, 8, fm(cif, 8, dt=F32))])
            blk(D0, 512, [("fm", 128 * j, 128, fm(dq[j], 128, dt=BF16, scale=128 ** -0.5)) for j in range(4)])
            blk(D0 + 512, 512, [("fm", 128 * j, 128, fm(dk[j], 128, dt=BF16)) for j in range(4)])
            blk(D0 + 1024, 512, [("tm", 0, 512, tm(dv, 512, dt=BF16))])
            gemm(hT, DC, T, blocks, 512)

        def phase_gates(l):
            with ExitStack() as _st:
                bgc = _st.enter_context(nc.sbuf_tensor(U("bgc"), [128, 4, DC], F32))
                S.dma("sp", lambda e: e.dma_start(out=bgc[:], in_=dr["b_gate_c"][l]), writes=["bias"])
                blocks = []
                for b in range(4):
                    for cb in range(4):
                        jobs = []
                        for j in range(4):
                            c = cb * 4 + j
                            jobs.append(("fm", 128 * j, 128, epi_simple(lambda t0, b=b, c=c: gT[b, :, c, t0:t0 + 512], dt=BF16, func=AF.Sigmoid,
                                                                       bias=bgc[:, b, c:c + 1])))
                        blocks.append((dr["w_gate"][l, b][:, cb * 512:(cb + 1) * 512], 512, jobs))
                gemm(hT, DC, T, blocks, 512)

        def phase_outproj(l):
            blocks = []
            for cb in range(4):
                jobs = [("fm", 128 * j, 128, epi_simple(lambda t0, c=cb * 4 + j: yT[:, c, t0:t0 + 512], dt=F32)) for j in range(4)]
                blocks.append((dr["w_out"][l][:, cb * 512:(cb + 1) * 512], 512, jobs))
            gemm(mT, DC, T, blocks, 512)

        def phase_ffn_up(l):
            blocks = []
            for cb in range(16):
                jobs = [("fm", 128 * j, 128, epi_simple(lambda t0, c=cb * 4 + j: fT[:, c, t0:t0 + 512], dt=BF16, post="relu2")) for j in range(4)]
                blocks.append((dr["w_up"][l][:, cb * 512:(cb + 1) * 512], 512, jobs))
            gemm(hT, DC, T, blocks, 512)

        def phase_ffn_down(l):
            blocks = []
            for c in range(DC):
                jobs = [("fm", 0, 128, epi_simple(lambda t0, c=c: yT[:, c, t0:t0 + 512], dt=F32))]
                blocks.append((dr["w_down"][l][:, c * 128:(c + 1) * 128], 128, jobs))
            gemm(fT, 64, min(T, 1024), blocks, 128)

        def phase_norm(gpost_ap, gnext_ap, final):
            with ExitStack() as _st:
                yt = _st.enter_context(nc.sbuf_tensor(U("yt"), [128, 2, DC, 512], F32))
                xt = _st.enter_context(nc.sbuf_tensor(U("xt"), [128, 2, DC, 512], F32))
                sq = _st.enter_context(nc.sbuf_tensor(U("sq"), [128, DC, 512], BF16))
                rs = _st.enter_context(nc.sbuf_tensor(U("rs"), [128, 512], F32))
                ht = _st.enter_context(nc.sbuf_tensor(U("ht"), [128, DC, 512], BF16))
                gpost = _st.enter_context(nc.sbuf_tensor(U("gpost"), [128, DC], F32))
                gcol = _st.enter_context(nc.sbuf_tensor(U("gcol"), [128, DC], F32))
                xo = _st.enter_context(nc.sbuf_tensor(U("xo"), [128, 2, D], F32))
                S.dma("sp", lambda e: e.dma_start(out=gpost[:], in_=gpost_ap), writes=["gpost"])
                if not final:
                    S.dma("sp", lambda e: e.dma_start(out=gcol[:], in_=gnext_ap), writes=["gcol"])

                def load(tg):
                    sl = tg % 2
                    S.dma("sp", lambda e: e.dma_start(out=yt[:, sl], in_=yT[:, :, tg * 512:(tg + 1) * 512]), writes=[("yt", sl)])
                    S.dma("sp", lambda e: e.dma_start(out=xt[:, sl], in_=xT[:, :, tg * 512:(tg + 1) * 512]), writes=[("xt", sl)])
                load(0)
                for tg in range(NT):
                    sl = tg % 2
                    if tg + 1 < NT:
                        load(tg + 1)
                    y = yt[:, sl]
                    xx = xt[:, sl]
                    for c4 in range(4):
                        S.op("act", lambda e, c4=c4, y=y: e.activation(out=sq[:, 4 * c4:4 * c4 + 4, :], in_=y[:, 4 * c4:4 * c4 + 4, :], func=AF.Square),
                             reads=[("yt", sl)], writes=[("sq", c4)])
                    for c in range(DC):
                        S.op("pe", lambda e, c=c: e.matmul(ps[6][:, 0:512], ones_b[:], sq[:, c, :], start=(c == 0), stop=(c == DC - 1)),
                             reads=[("sq", c // 4), "ones_b"], writes=[("ps", 6)])
                    S.op("act", lambda e: e.activation(out=rs[:], in_=ps[6][:, 0:512], func=AF.Sqrt, bias=EPS, scale=1.0 / D), reads=[("ps", 6)], writes=["rs"])
                    S.op("dve", lambda e: e.reciprocal(out=rs[:], in_=rs[:]), reads=["rs"], writes=["rs"])
                    for c in range(DC):
                        S.op("dve", lambda e, c=c, y=y: e.scalar_tensor_tensor(out=y[:, c, :], in0=y[:, c, :], scalar=gpost[:, c:c + 1], in1=rs[:],
                                                                               op0=ALU.mult, op1=ALU.mult), reads=[("yt", sl), "rs", "gpost"], writes=[("yt", sl)])
                        S.op("pool", lambda e, c=c, y=y, xx=xx: e.tensor_tensor(out=xx[:, c, :], in0=xx[:, c, :], in1=y[:, c, :], op=ALU.add),
                             reads=[("yt", sl), ("xt", sl)], writes=[("xt", sl)])
                    if not final:
                        S.dma("sp", lambda e, tg=tg, xx=xx: e.dma_start(out=xT[:, :, tg * 512:(tg + 1) * 512], in_=xx), reads=[("xt", sl)])
                        norm_tile(xx, ("xt", sl), gcol, 512, ht, "ht", sq, rs, 7)
                        S.dma("sp", lambda e, tg=tg: e.dma_start(out=hT[:, :, tg * 512:(tg + 1) * 512], in_=ht[:]), reads=["ht"])
                    else:
                        for tb in range(4):
                            osl = tb % 2
                            for cg in range(4):
                                b = next_bank()
                                for j in range(4):
                                    c = cg * 4 + j
                                    S.op("pe", lambda e, b=b, j=j, c=c, tb=tb, xx=xx: e.transpose(ps[b][:, 128 * j:128 * j + 128], xx[:, c, 128 * tb:128 * tb + 128], ident_f[:]),
                                         reads=[("xt", sl), "ident_f"], writes=[("ps", b)])
                                eng = evac_eng()
                                if eng == "act":
                                    S.op("act", lambda e, b=b, cg=cg, osl=osl: e.activation(out=xo[:, osl, 512 * cg:512 * cg + 512], in_=ps[b][:, :], func=AF.Copy),
                                         reads=[("ps", b)], writes=[("xo", osl)])
                                else:
                                    S.op("dve", lambda e, b=b, cg=cg, osl=osl: e.tensor_copy(out=xo[:, osl, 512 * cg:512 * cg + 512], in_=ps[b][:, :]),
                                         reads=[("ps", b)], writes=[("xo", osl)])
                            r0 = tg * 512 + tb * 128
                            S.dma("sp", lambda e, r0=r0, osl=osl: e.dma_start(out=out[r0:r0 + 128, :], in_=xo[:, osl, :]), reads=[("xo", osl)])
                S.emit()

        def phase_merge(l):
            with ExitStack() as _st:
                wb = _st.enter_context(nc.sbuf_tensor(U("wb"), [128, 4, 4, D], BF16))
                brs = _st.enter_context(nc.sbuf_tensor(U("brs"), [128, 2, DC, 512], BF16))
                gs = _st.enter_context(nc.sbuf_tensor(U("gs"), [128, 2, 4, 4, 512], BF16))
                tt = _st.enter_context(nc.sbuf_tensor(U("tt"), [128, 2, 4, 512], F32))
                mt = _st.enter_context(nc.sbuf_tensor(U("mt"), [128, DC, 512], BF16))
                for b in range(4):
                    S.dma("pool", [lambda e, b=b, n0=n0: e.dma_start(out=wb[:, b, :, n0:n0 + 512], in_=dr["w_branch"][l, b][:, n0:n0 + 512].rearrange("(c p) n -> p c n", p=128))
                                   for n0 in range(0, D, 512)], writes=[("wb", b)])

                def loadbr(tg):
                    S.dma("sp", lambda e: e.dma_start(out=brs[:, tg % 2], in_=brT[:, :, tg * 512:(tg + 1) * 512]), writes=[("brs", tg % 2)])

                def loadg(i):
                    tg, cg = divmod(i, 4)
                    sl = i % 2
                    S.dma("sp", [lambda e, b=b: e.dma_start(out=gs[:, sl, b], in_=gT[b, :, 4 * cg:4 * cg + 4, tg * 512:(tg + 1) * 512]) for b in range(4)],
                          writes=[("gs", sl)])
                loadbr(0)
                loadg(0)
                for tg in range(NT):
                    if tg + 1 < NT:
                        loadbr(tg + 1)
                    for cg in range(4):
                        i = tg * 4 + cg
                        if i + 1 < NT * 4:
                            loadg(i + 1)
                        for j in range(4):
                            c = cg * 4 + j
                            pp = c % 2
                            for b in range(4):
                                bk = 4 * pp + b
                                for k in range(4):
                                    S.op("pe", lambda e, b=b, bk=bk, k=k, c=c, tg=tg: e.matmul(ps[bk][:, 0:512], wb[:, b, k, 128 * c:128 * c + 128], brs[:, tg % 2, 4 * b + k, :],
                                                                                       start=(k == 0), stop=(k == 3)),
                                         reads=[("wb", b), ("brs", tg % 2)], writes=[("ps", bk)])
                                S.op("dve", lambda e, b=b, bk=bk, j=j, i=i, pp=pp: e.tensor_tensor(out=tt[:, pp, b, :], in0=ps[bk][:, 0:512], in1=gs[:, i % 2, b, j, :], op=ALU.mult),
                                     reads=[("ps", bk), ("gs", i % 2)], writes=[("tt", pp, b)])
                            S.op("dve", lambda e, pp=pp: e.tensor_tensor(out=tt[:, pp, 0, :], in0=tt[:, pp, 0, :], in1=tt[:, pp, 1, :], op=ALU.add), reads=[("tt", pp, 0), ("tt", pp, 1)], writes=[("tt", pp, 0)])
                            S.op("pool", lambda e, pp=pp: e.tensor_tensor(out=tt[:, pp, 2, :], in0=tt[:, pp, 2, :], in1=tt[:, pp, 3, :], op=ALU.add), reads=[("tt", pp, 2), ("tt", pp, 3)], writes=[("tt", pp, 2)])
                            S.op("pool", lambda e, c=c, pp=pp: e.tensor_tensor(out=mt[:, c, :], in0=tt[:, pp, 0, :], in1=tt[:, pp, 2, :], op=ALU.add), reads=[("tt", pp, 0), ("tt", pp, 2)], writes=["mt"])
                    S.dma("sp", lambda e, tg=tg: e.dma_start(out=mT[:, :, tg * 512:(tg + 1) * 512], in_=mt[:]), reads=["mt"])
                S.emit()

        def head_norm(x, xkey, P, gain, gkey, outb, okey, sqt, ss, tag):
            S.op("dve", lambda e: e.tensor_tensor(out=sqt, in0=x, in1=x, op=ALU.mult), reads=[xkey], writes=[tag + "sq"])
            S.op("dve", lambda e: e.tensor_reduce(out=ss, in_=sqt, axis=AX.X, op=ALU.add), reads=[tag + "sq"], writes=[tag + "ss"])
            S.op("act", lambda e: e.activation(out=ss, in_=ss, func=AF.Ln, bias=EPS, scale=1.0 / 128), reads=[tag + "ss"], writes=[tag + "ss"])
            S.op("act", lambda e: e.activation(out=ss, in_=ss, func=AF.Exp, scale=-0.5), reads=[tag + "ss"], writes=[tag + "ss"])
            S.op("dve", lambda e: e.tensor_tensor(out=x, in0=x, in1=ss.unsqueeze(2).to_broadcast([P, 4, 128]), op=ALU.mult), reads=[xkey, tag + "ss"], writes=[xkey])
            S.op("dve", lambda e: e.tensor_tensor(out=outb, in0=x, in1=gain, op=ALU.mult), reads=[xkey, gkey], writes=[okey])

        def mixer_D(l):
            with ExitStack() as _st:
                q_sb = _st.enter_context(nc.sbuf_tensor(U("q_sb"), [128, 2, T], BF16))
                k_sb = _st.enter_context(nc.sbuf_tensor(U("k_sb"), [128, 2, T], BF16))
                v_sb = _st.enter_context(nc.sbuf_tensor(U("v_sb"), [128, NB, 4, 130], BF16))
                pT = _st.enter_context(nc.sbuf_tensor(U("pT"), [128, 2, 8, 512], BF16))
                bt = _st.enter_context(nc.sbuf_tensor(U("bt"), [128, 3, 4, 128], F32))
                dmk = _st.enter_context(nc.sbuf_tensor(U("dmk"), [128, 2, 128], F32))
                bfar = _st.enter_context(nc.sbuf_tensor(U("bfar"), [128, 4], F32))
                tmp = _st.enter_context(nc.sbuf_tensor(U("tmp"), [128, 4, 128], F32))
                rc = _st.enter_context(nc.sbuf_tensor(U("rc"), [128, 4], F32))
                o_sb = _st.enter_context(nc.sbuf_tensor(U("o_sb"), [128, 2, 4, 128], BF16))
                stg = _st.enter_context(nc.sbuf_tensor(U("stg"), [128, 2, 512], BF16))
                S.dma("sp", lambda e: e.dma_start(out=bt[:, 0:2], in_=dr["d_bias"][l]), writes=["bt"])
                S.dma("sp", lambda e: e.dma_start(out=dmk[:], in_=dr["c_dmask"]), writes=["dmk"])
                S.dma("sp", lambda e: e.dma_start(out=bfar[:], in_=dr["d_far"][l]), writes=["bfar"])
                S.op("pool", lambda e: e.memset(v_sb[:, :, :, 128:130], 1.0), writes=["v1"])
                S.dma("sp", [lambda e, b0=b0: e.dma_start(out=v_sb[:, b0, :, 0:128], in_=dv[b0 * 128:(b0 + 1) * 128, :].rearrange("p (h d) -> p h d", h=4))
                             for b0 in range(NB)], writes=["v"])
                for h in range(4):
                    S.op("dve", lambda e, h=h: e.tensor_tensor(out=bt[:, 0, h, :], in0=bt[:, 0, h, :], in1=dmk[:, 0, :], op=ALU.add), reads=["bt", "dmk"], writes=["bt"])
                    S.op("dve", lambda e, h=h: e.tensor_scalar(out=bt[:, 2, h, :], in0=dmk[:, 1, :], scalar1=bfar[:, h:h + 1], scalar2=None, op0=ALU.add),
                         reads=["dmk", "bfar"], writes=["bt"])

                def loadqk(h):
                    S.dma("sp", lambda e: e.dma_start(out=q_sb[:, h % 2, :], in_=dq[h]), writes=[("q", h % 2)])
                    S.dma("sp", lambda e: e.dma_start(out=k_sb[:, h % 2, :], in_=dk[h]), writes=[("k", h % 2)])
                loadqk(0)
                it = 0
                for h in range(4):
                    if h + 1 < 4:
                        loadqk(h + 1)
                    hs = h % 2
                    for sb in range(NT):
                        Q0 = sb * 512
                        pb = it % 2
                        it += 1
                        for j in range(8):
                            K0 = Q0 - 512 + 128 * j
                            if K0 < 0:
                                continue
                            ilo, ihi = max(0, j - 4), min(3, j)
                            c0, c1 = 128 * ilo, 128 * (ihi + 1)
                            b = next_bank(3)
                            S.op("pe", lambda e, b=b, K0=K0, c0=c0, c1=c1, Q0=Q0, hs=hs: e.matmul(ps[b][:, c0:c1], k_sb[:, hs, K0:K0 + 128], q_sb[:, hs, Q0 + c0:Q0 + c1],
                                                                                               start=True, stop=True),
                                 reads=[("q", hs), ("k", hs)], writes=[("ps", b)])
                            i = ilo
                            while i <= ihi:
                                dl = i + 4 - j
                                if dl in (0, 1, 4):
                                    bi = {0: 0, 1: 1, 4: 2}[dl]
                                    S.op("dve", lambda e, b=b, i=i, bi=bi, h=h: e.tensor_tensor(out=tmp[:, i, :], in0=ps[b][:, 128 * i:128 * i + 128], in1=bt[:, bi, h, :], op=ALU.add),
                                         reads=[("ps", b), "bt"], writes=[("tmp", i)])
                                    S.op("act", lambda e, i=i, pb=pb, j=j: e.activation(out=pT[:, pb, j, 128 * i:128 * i + 128], in_=tmp[:, i, :], func=AF.Exp),
                                         reads=[("tmp", i)], writes=[("pT", pb, j, i)])
                                    i += 1
                                else:
                                    i2 = i
                                    while i2 + 1 <= ihi and (i2 + 1 + 4 - j) in (2, 3):
                                        i2 += 1
                                    S.op("act", lambda e, b=b, i=i, i2=i2, pb=pb, j=j, h=h: e.activation(out=pT[:, pb, j, 128 * i:128 * (i2 + 1)], in_=ps[b][:, 128 * i:128 * (i2 + 1)],
                                                                                                         func=AF.Exp, bias=bfar[:, h:h + 1]),
                                         reads=[("ps", b), "bfar"], writes=[("pT", pb, j, ii) for ii in range(i, i2 + 1)])
                                    i = i2 + 1
                        for i in range(4):
                            js = [j for j in range(8) if 0 <= i + 4 - j <= 4 and Q0 - 512 + 128 * j >= 0]
                            pvb = 3 + i
                            for n_, j in enumerate(js):
                                kb = (Q0 - 512 + 128 * j) // 128
                                S.op("pe", lambda e, pvb=pvb, pb=pb, j=j, i=i, kb=kb, h=h, n_=n_, js=js: e.matmul(
                                    ps[pvb][:, 0:129], pT[:, pb, j, 128 * i:128 * i + 128], v_sb[:, kb, h, 0:129], start=(n_ == 0), stop=(n_ == len(js) - 1)),
                                    reads=[("pT", pb, j, i), "v", "v1"], writes=[("ps", pvb)])
                            S.op("dve", lambda e, pvb=pvb, i=i: e.reciprocal(out=rc[:, i:i + 1], in_=ps[pvb][:, 128:129]), reads=[("ps", pvb)], writes=[("rc", i)])
                            S.op("act", lambda e, pvb=pvb, i=i, pb=pb: e.activation(out=o_sb[:, pb, i, :], in_=ps[pvb][:, 0:128], func=AF.Identity, scale=rc[:, i:i + 1]),
                                 reads=[("ps", pvb), ("rc", i)], writes=[("o", pb, i)])
                        tb_ = ps[7][:, 0:256].bitcast(BF16)
                        for i in range(4):
                            S.op("pe", lambda e, i=i, pb=pb: e.transpose(tb_[:, 128 * i:128 * i + 128], o_sb[:, pb, i, :], ident_b[:]),
                                 reads=[("o", pb, i), "ident_b"], writes=[("ps", 7)])
                        S.op("dve", lambda e, pb=pb: e.tensor_copy(out=stg[:, pb, :], in_=tb_), reads=[("ps", 7)], writes=[("stg", pb)])
                        S.dma("sp", lambda e, pb=pb, h=h, Q0=Q0: e.dma_start(out=brT[:, 12 + h, Q0:Q0 + 512], in_=stg[:, pb, :]), reads=[("stg", pb)])
                S.emit()

        def mixer_A(l):
            lam_init = 0.8 - 0.6 * math.exp(-0.3 * l)
            with ExitStack() as _st:
                qa = _st.enter_context(nc.sbuf_tensor(U("qa"), [68, 2, 2, T], BF16))
                ka = _st.enter_context(nc.sbuf_tensor(U("ka"), [68, 2, 2, T], BF16))
                v_sb = _st.enter_context(nc.sbuf_tensor(U("v_sb"), [128, NB, 4, 130], BF16))
                pT = _st.enter_context(nc.sbuf_tensor(U("pT"), [128, 3, 512], BF16))
                cdg = _st.enter_context(nc.sbuf_tensor(U("cdg"), [128, 4, 128], F32))
                tmp = _st.enter_context(nc.sbuf_tensor(U("tmp"), [128, 2, 128], F32))
                rc = _st.enter_context(nc.sbuf_tensor(U("rc"), [128, 4], F32))
                om = _st.enter_context(nc.sbuf_tensor(U("om"), [128, 2, 4, 128], F32))
                lam = _st.enter_context(nc.sbuf_tensor(U("lam"), [128, 256], F32))
                lsc = _st.enter_context(nc.sbuf_tensor(U("lsc"), [128, 4], F32))
                gain = _st.enter_context(nc.sbuf_tensor(U("gain"), [128, 512], F32))
                sqt = _st.enter_context(nc.sbuf_tensor(U("sqt"), [128, 4, 128], F32))
                ss = _st.enter_context(nc.sbuf_tensor(U("ss"), [128, 4], F32))
                o_bf = _st.enter_context(nc.sbuf_tensor(U("o_bf"), [128, 4, 128], BF16))
                stg = _st.enter_context(nc.sbuf_tensor(U("stg"), [128, 2, 512], BF16))
                S.dma("sp", lambda e: e.dma_start(out=cdg[:], in_=dr["c_cdiag"]), writes=["cdg"])
                S.dma("sp", lambda e: e.dma_start(out=lam[:], in_=dr["diff_lambda"][l, 0].partition_broadcast(128)), writes=["lam"])
                S.dma("sp", lambda e: e.dma_start(out=gain[:], in_=dr["diff_norm"][l, 0].partition_broadcast(128)), writes=["gain"])
                S.op("pool", lambda e: e.memset(v_sb[:, :, :, 128:130], 1.0), writes=["v1"])
                S.dma("sp", [lambda e, b0=b0: e.dma_start(out=v_sb[:, b0, :, 0:128], in_=av[b0 * 128:(b0 + 1) * 128, :].rearrange("p (h d) -> p h d", h=4))
                             for b0 in range(NB)], writes=["v"])
                S.op("dve", lambda e: e.tensor_tensor(out=lam[:, 0:64], in0=lam[:, 0:64], in1=lam[:, 64:128], op=ALU.mult), reads=["lam"], writes=["lam"])
                S.op("dve", lambda e: e.tensor_tensor(out=lam[:, 128:192], in0=lam[:, 128:192], in1=lam[:, 192:256], op=ALU.mult), reads=["lam"], writes=["lam"])
                S.op("dve", lambda e: e.tensor_reduce(out=lsc[:, 0:2], in_=lam[:, :].rearrange("p (a b) -> p a b", a=2)[:, :, 0:64], axis=AX.X, op=ALU.add),
                     reads=["lam"], writes=["lsc"])
                S.op("act", lambda e: e.activation(out=lsc[:, 0:2], in_=lsc[:, 0:2], func=AF.Exp), reads=["lsc"], writes=["lsc"])
                S.op("dve", lambda e: e.scalar_tensor_tensor(out=lsc[:, 2:3], in0=lsc[:, 1:2], scalar=-lam_init, in1=lsc[:, 0:1], op0=ALU.add, op1=ALU.subtract),
                     reads=["lsc"], writes=["lsc"])
                S.op("dve", lambda e: e.tensor_scalar(out=gain[:], in0=gain[:], scalar1=float(1.0 - lam_init), scalar2=None, op0=ALU.mult), reads=["gain"], writes=["gain"])

                def loadqk(h):
                    hb = h % 2
                    for m in range(2):
                        S.dma("sp", [lambda e, m=m: e.dma_start(out=qa[0:64, hb, m, :], in_=aq[2 * h + m]),
                                     lambda e, m=m: e.dma_start(out=qa[64:68, hb, m, :], in_=dr["c_augq"][h]),
                                     lambda e, m=m: e.dma_start(out=ka[0:64, hb, m, :], in_=ak[2 * h + m]),
                                     lambda e, m=m: e.dma_start(out=ka[64:68, hb, m, :], in_=dr["c_augk"][h])], writes=[("qk", hb, m)])
                loadqk(0)
                LA = 2
                qkb = [0, 1, 7]
                st_ = {"n": 0, "it": 0, "pend": None, "cd": 0}
                slot_of = {}

                def qk_exp(h, hb, sb, m, kb):
                    Q0 = sb * 512
                    K0 = 128 * kb
                    n = st_["n"]
                    st_["n"] += 1
                    b = qkb[n % 3]
                    psl = n % 3
                    slot_of[(h, sb, m, kb)] = psl
                    if K0 < Q0:
                        S.op("pe", lambda e: e.matmul(ps[b][:, 0:512], ka[:, hb, m, K0:K0 + 128], qa[:, hb, m, Q0:Q0 + 512], start=True, stop=True),
                             reads=[("qk", hb, m)], writes=[("ps", b)])
                        S.op("act", lambda e: e.activation(out=pT[:, psl, :], in_=ps[b][:, 0:512], func=AF.Exp), reads=[("ps", b)], writes=[("pT", psl)])
                    else:
                        jl = (K0 - Q0) // 128
                        c0 = 128 * jl
                        S.op("pe", lambda e: e.matmul(ps[b][:, c0:512], ka[:, hb, m, K0:K0 + 128], qa[:, hb, m, Q0 + c0:Q0 + 512], start=True, stop=True),
                             reads=[("qk", hb, m)], writes=[("ps", b)])
                        ts_ = st_["it"] % 2
                        st_["it"] += 1
                        S.op("dve", lambda e: e.tensor_tensor(out=tmp[:, ts_, :], in0=ps[b][:, c0:c0 + 128], in1=cdg[:, h, :], op=ALU.add),
                             reads=[("ps", b), "cdg"], writes=[("tmp", ts_)])
                        S.op("act", lambda e: e.activation(out=pT[:, psl, c0:c0 + 128], in_=tmp[:, ts_, :], func=AF.Exp),
                             reads=[("tmp", ts_)], writes=[("pT", psl)])
                        if jl < 3:
                            S.op("act", lambda e: e.activation(out=pT[:, psl, c0 + 128:512], in_=ps[b][:, c0 + 128:512], func=AF.Exp),
                                 reads=[("ps", b)], writes=[("pT", psl)])

                def pe_tail(h, sb):
                    Q0 = sb * 512
                    tb_ = ps[6][:, 0:256].bitcast(BF16)
                    for i in range(4):
                        S.op("pe", lambda e, i=i: e.transpose(tb_[:, 128 * i:128 * i + 128], o_bf[:, i, :], ident_b[:]), reads=["o_bf", "ident_b"], writes=[("ps", 6)])
                    sg = (h * NT + sb) % 2
                    S.op("dve", lambda e: e.tensor_copy(out=stg[:, sg, :], in_=tb_), reads=[("ps", 6)], writes=[("stg", sg)])
                    S.dma("sp", lambda e: e.dma_start(out=brT[:, h, Q0:Q0 + 512], in_=stg[:, sg, :]), reads=[("stg", sg)])

                def pv(h, hb, sb, m, kb):
                    Q0 = sb * 512
                    K0 = 128 * kb
                    psl = slot_of.pop((h, sb, m, kb))
                    jl = 0 if K0 < Q0 else (K0 - Q0) // 128
                    for i in range(jl, 4):
                        S.op("pe", lambda e, i=i: e.matmul(ps[2 + i][:, 0:129], pT[:, psl, 128 * i:128 * i + 128], v_sb[:, kb, h, 0:129],
                                                          start=(kb == 0), stop=(kb == 4 * sb + i)),
                             reads=[("pT", psl), "v", "v1"], writes=[("ps", 2 + i)])
                    if kb != 4 * sb + 3:
                        return
                    for i in range(4):
                        S.op("dve", lambda e, i=i: e.reciprocal(out=rc[:, i:i + 1], in_=ps[2 + i][:, 128:129]), reads=[("ps", 2 + i)], writes=[("rc", i)])
                        if m == 0:
                            S.op("act", lambda e, i=i: e.activation(out=om[:, 0, i, :], in_=ps[2 + i][:, 0:128], func=AF.Identity, scale=rc[:, i:i + 1]),
                                 reads=[("ps", 2 + i), ("rc", i)], writes=[("om", 0)])
                        else:
                            S.op("dve", lambda e, i=i: e.tensor_scalar(out=om[:, 1, i, :], in0=ps[2 + i][:, 0:128], scalar1=rc[:, i:i + 1], scalar2=lsc[:, 2:3],
                                                                       op0=ALU.mult, op1=ALU.mult), reads=[("ps", 2 + i), ("rc", i), "lsc"], writes=[("om", 1)])
                    if m == 1:
                        S.op("pool", lambda e: e.tensor_tensor(out=om[:, 0], in0=om[:, 0], in1=om[:, 1], op=ALU.add), reads=[("om", 0), ("om", 1)], writes=[("om", 0)])
                        head_norm(om[:, 0], ("om", 0), 128, gain[:, 128 * h:128 * h + 128].unsqueeze(1).to_broadcast([128, 4, 128]), "gain", o_bf[:], "o_bf", sqt[:], ss[:], "A")
                        st_["pend"] = (h, sb)
                        st_["cd"] = 3

                for h in range(4):
                    if h + 1 < 4:
                        loadqk(h + 1)
                    hb = h % 2
                    items = [(sb, m, kb) for sb in range(NT) for m in range(2) for kb in range(4 * sb + 4)]
                    for j in range(min(LA, len(items))):
                        qk_exp(h, hb, *items[j])
                    for idx, itm in enumerate(items):
                        if idx + LA < len(items):
                            qk_exp(h, hb, *items[idx + LA])
                        pv(h, hb, *itm)
                        if st_["pend"] is not None:
                            if st_["cd"] == 0 or idx == len(items) - 1:
                                pe_tail(*st_["pend"])
                                st_["pend"] = None
                            else:
                                st_["cd"] -= 1
                S.emit()

        def mixer_B(l):
            TS = min(T, 1024)
            NCS = TS // 64
            with ExitStack() as _st:
                q_sb = _st.enter_context(nc.sbuf_tensor(U("q_sb"), [64, 4, TS], F32))
                k_sb = _st.enter_context(nc.sbuf_tensor(U("k_sb"), [64, 4, TS], F32))
                kT_sb = _st.enter_context(nc.sbuf_tensor(U("kT_sb"), [64, NCS, 256], F32))
                v_sb = _st.enter_context(nc.sbuf_tensor(U("v_sb"), [64, NCS, 512], BF16))
                r_sb = _st.enter_context(nc.sbuf_tensor(U("r_sb"), [64, NCS, 512], F32))
                g_sb = _st.enter_context(nc.sbuf_tensor(U("g_sb"), [17, TS], F32))
                wg = _st.enter_context(nc.sbuf_tensor(U("wg"), [17, 256], F32))
                cst = _st.enter_context(nc.sbuf_tensor(U("cst"), [64, 3, 64], F32))
                gain = _st.enter_context(nc.sbuf_tensor(U("gain"), [64, 512], F32))
                e1 = _st.enter_context(nc.sbuf_tensor(U("e1"), [64, 256], F32))
                la = _st.enter_context(nc.sbuf_tensor(U("la"), [64, 256], F32))
                kte = _st.enter_context(nc.sbuf_tensor(U("kte"), [64, 256], F32))
                ktl = _st.enter_context(nc.sbuf_tensor(U("ktl"), [64, 2, 256], BF16))
                ecum = _st.enter_context(nc.sbuf_tensor(U("ecum"), [64, 2, 4, 64], F32))
                eneg = _st.enter_context(nc.sbuf_tensor(U("eneg"), [64, 4, 64], F32))
                qd = _st.enter_context(nc.sbuf_tensor(U("qd"), [64, 2, 4, 64], BF16))
                ki = _st.enter_context(nc.sbuf_tensor(U("ki"), [64, 4, 64], BF16))
                att = _st.enter_context(nc.sbuf_tensor(U("att"), [64, 2, 4, 64], BF16))
                Sf = _st.enter_context(nc.sbuf_tensor(U("Sf"), [64, 4, 128], F32))
                Sb = _st.enter_context(nc.sbuf_tensor(U("Sb"), [64, 4, 128], BF16))
                of = _st.enter_context(nc.sbuf_tensor(U("of"), [64, 4, 128], F32))
                gr = _st.enter_context(nc.sbuf_tensor(U("gr"), [64, 2, 4, 128], F32))
                sqt = _st.enter_context(nc.sbuf_tensor(U("sqt"), [64, 4, 128], F32))
                ss = _st.enter_context(nc.sbuf_tensor(U("ss"), [64, 4], F32))
                o_bf = _st.enter_context(nc.sbuf_tensor(U("o_bf"), [64, 4, 128], BF16))
                stg = _st.enter_context(nc.sbuf_tensor(U("stg"), [128, 2, 4, 512], BF16))
                S.dma("sp", lambda e: e.dma_start(out=wg[:], in_=dr["gla_wg"][l]), writes=["wg"])
                S.dma("sp", [lambda e: e.dma_start(out=cst[:, 0, :], in_=dr["c_triu"]), lambda e: e.dma_start(out=cst[:, 1, :], in_=dr["c_trisl"]),
                             lambda e: e.dma_start(out=cst[:, 2, :], in_=dr["c_mask01"])], writes=["cst"])
                S.dma("sp", lambda e: e.dma_start(out=gain[:], in_=dr["gla_norm"][l, 0].partition_broadcast(64)), writes=["gain"])
                S.op("dve", lambda e: e.memset(Sf[:], 0.0), writes=["Sf"])
                S.op("pool", lambda e: e.memset(Sb[:], 0.0), writes=["Sb"])
                for ts in range(T // TS):
                    t0s = ts * TS
                    S.dma("sp", [lambda e, h=h, t0s=t0s: e.dma_start(out=q_sb[:, h, :], in_=bq[h][:, t0s:t0s + TS]) for h in range(4)] +
                          [lambda e, h=h, t0s=t0s: e.dma_start(out=k_sb[:, h, :], in_=bk[h][:, t0s:t0s + TS]) for h in range(4)], writes=["qk"])
                    S.dma("sp", lambda e, t0s=t0s: e.dma_start(out=kT_sb[:], in_=bkT[t0s:t0s + TS, :].rearrange("(c p) n -> p c n", p=64)), writes=["kT"])
                    S.dma("sp", lambda e, t0s=t0s: e.dma_start(out=v_sb[:], in_=bv[t0s:t0s + TS, :].rearrange("(c p) n -> p c n", p=64)), writes=["v"])
                    S.dma("sp", lambda e, t0s=t0s: e.dma_start(out=r_sb[:], in_=br[t0s:t0s + TS, :].rearrange("(c p) n -> p c n", p=64)), writes=["r"])
                    S.dma("sp", [lambda e, t0s=t0s: e.dma_start(out=g_sb[0:16, :], in_=bg[:, t0s:t0s + TS]),
                                 lambda e: e.dma_start(out=g_sb[16:17, :], in_=dr["c_onesrow"][:, 0:TS])], writes=["g"])
                    def b_stage1(c):
                        t0 = c * 64
                        p = c % 2
                        S.op("pe", lambda e: e.matmul(ps[0][0:64, 0:256], g_sb[:, t0:t0 + 64], wg[:], start=True, stop=True), reads=["g", "wg"], writes=[("ps", 0)])
                        S.op("act", lambda e: e.activation(out=e1[:], in_=ps[0][0:64, 0:256], func=AF.Exp, scale=-1.0), reads=[("ps", 0)], writes=["e1"])
                        S.op("act", lambda e: e.activation(out=la[:], in_=e1[:], func=AF.Ln, bias=1.0), reads=["e1"], writes=["la"])
                        S.op("pe", lambda e: e.matmul(ps[1][0:64, 0:256], cst[:, 1, :], la[:], start=True, stop=True), reads=["la", "cst"], writes=[("ps", 1)])
                        for h in range(4):
                            S.op("pe", lambda e, h=h: e.matmul(ps[2][0:64, 64 * h:64 * h + 64], la[:, 64 * h:64 * h + 64], cst[:, 0, :], start=True, stop=True),
                                 reads=["la", "cst"], writes=[("ps", 2)])
                        S.op("act", lambda e: e.activation(out=kte[:], in_=ps[1][0:64, 0:256], func=AF.Exp), reads=[("ps", 1)], writes=["kte"])
                        S.op("dve", lambda e: e.tensor_tensor(out=ktl[:, p, :], in0=kT_sb[:, c, :], in1=kte[:], op=ALU.mult), reads=["kT", "kte"], writes=[("ktl", p)])
                        S.op("act", lambda e: e.activation(out=ecum[:, p], in_=ps[2][0:64, 0:256].rearrange("p (h t) -> p h t", h=4), func=AF.Exp), reads=[("ps", 2)], writes=[("ecum", p)])
                        S.op("act", lambda e: e.activation(out=eneg[:], in_=ps[2][0:64, 0:256].rearrange("p (h t) -> p h t", h=4), func=AF.Exp, scale=-1.0),
                             reads=[("ps", 2)], writes=["eneg"])
                        S.op("dve", lambda e: e.tensor_tensor(out=qd[:, p], in0=q_sb[:, :, t0:t0 + 64], in1=ecum[:, p], op=ALU.mult), reads=["qk", ("ecum", p)], writes=[("qd", p)])
                        S.op("pool", lambda e: e.tensor_tensor(out=ki[:], in0=k_sb[:, :, t0:t0 + 64], in1=eneg[:], op=ALU.mult), reads=["qk", "eneg"], writes=["ki"])
                        for h in range(4):
                            S.op("pe", lambda e, h=h: e.matmul(ps[3][0:64, 64 * h:64 * h + 64], ki[:, h, :], qd[:, p, h, :], start=True, stop=True), reads=["ki", ("qd", p)], writes=[("ps", 3)])
                        S.op("dve", lambda e: e.tensor_tensor(out=att[:, p], in0=ps[3][0:64, 0:256].rearrange("p (h t) -> p h t", h=4),
                                                              in1=cst[:, 2, :].unsqueeze(1).to_broadcast([64, 4, 64]), op=ALU.mult), reads=[("ps", 3), "cst"], writes=[("att", p)])
                        S.op("pool", lambda e: e.tensor_tensor(out=gr[:, p], in0=r_sb[:, c, :].rearrange("p (h d) -> p h d", h=4), in1=gain[:, :].rearrange("p (h d) -> p h d", h=4), op=ALU.mult),
                             reads=["r", "gain"], writes=[("gr", p)])

                    def b_stage2(c):
                        t0 = c * 64
                        tg = t0s + t0
                        p = c % 2
                        for h in range(4):
                            S.op("pe", lambda e, h=h: e.matmul(ps[4][0:64, 128 * h:128 * h + 128], att[:, p, h, :], v_sb[:, c, 128 * h:128 * h + 128], start=True, stop=False),
                                 reads=[("att", p), "v"], writes=[("ps", 4)])
                            S.op("pe", lambda e, h=h: e.matmul(ps[4][0:64, 128 * h:128 * h + 128], qd[:, p, h, :], Sb[:, h, :], start=False, stop=True),
                                 reads=[("qd", p), "Sb"], writes=[("ps", 4)])
                        for h in range(4):
                            S.op("pe", lambda e, h=h: e.matmul(ps[5][0:64, 128 * h:128 * h + 128], ktl[:, p, 64 * h:64 * h + 64], v_sb[:, c, 128 * h:128 * h + 128], start=True, stop=True),
                                 reads=[("ktl", p), "v"], writes=[("ps", 5)])
                        S.op("dve", lambda e: e.tensor_tensor(out=Sf[:], in0=Sf[:], in1=ecum[:, p, :, 63:64].to_broadcast([64, 4, 128]), op=ALU.mult), reads=["Sf", ("ecum", p)], writes=["Sf"])
                        S.op("dve", lambda e: e.tensor_tensor(out=Sf[:], in0=Sf[:], in1=ps[5][0:64, :].rearrange("p (h d) -> p h d", h=4), op=ALU.add), reads=["Sf", ("ps", 5)], writes=["Sf"])
                        S.op("act", lambda e: e.activation(out=Sb[:], in_=Sf[:], func=AF.Copy), reads=["Sf"], writes=["Sb"])
                        S.op("act", lambda e: e.activation(out=of[:], in_=ps[4][0:64, :].rearrange("p (h d) -> p h d", h=4), func=AF.Copy), reads=[("ps", 4)], writes=["of"])
                        head_norm(of[:], "of", 64, gr[:, p], ("gr", p), o_bf[:], "o_bf", sqt[:], ss[:], "B")
                        cc = (tg // 64) % 8
                        sg = (tg // 512) % 2
                        tb_ = ps[6][:, 0:128].bitcast(BF16)
                        for h in range(4):
                            S.op("pe", lambda e, h=h: e.transpose(tb_[:, 64 * h:64 * h + 64], o_bf[:, h, :], ident_b[0:64, 0:64]), reads=["o_bf", "ident_b"], writes=[("ps", 6)])
                        S.op("act", lambda e: e.activation(out=stg[:, sg, :, 64 * cc:64 * cc + 64], in_=tb_.rearrange("p (h t) -> p h t", h=4), func=AF.Copy),
                             reads=[("ps", 6)], writes=[("stg", sg)])
                        if cc == 7:
                            T0 = (tg // 512) * 512
                            S.dma("sp", lambda e: e.dma_start(out=brT[:, 4:8, T0:T0 + 512], in_=stg[:, sg]), reads=[("stg", sg)])

                    b_stage1(0)
                    for c in range(NCS):
                        if c + 1 < NCS:
                            b_stage1(c + 1)
                        b_stage2(c)
                S.emit()

        def mixer_C(l):
            with ExitStack() as _st:
                xc = _st.enter_context(nc.sbuf_tensor(U("xc"), [128, 2, 515], F32))
                acc = _st.enter_context(nc.sbuf_tensor(U("acc"), [128, 2, 512], F32))
                yb = _st.enter_context(nc.sbuf_tensor(U("yb"), [128, 2, 512], BF16))
                cw = _st.enter_context(nc.sbuf_tensor(U("cw"), [128, 8, 4], F32))
                cb = _st.enter_context(nc.sbuf_tensor(U("cb"), [128, 8], F32))
                S.dma("sp", lambda e: e.dma_start(out=cw[:], in_=dr["conv_w"][l]), writes=["cw"])
                S.dma("sp", lambda e: e.dma_start(out=cb[:], in_=dr["conv_b"][l]), writes=["cw"])
                n = 0
                for cc in range(8):
                    for tg in range(NT):
                        sl = n % 2
                        n += 1
                        t0 = tg * 512
                        if tg == 0:
                            S.op("pool", lambda e, sl=sl: e.memset(xc[:, sl, 0:3], 0.0), writes=[("xc", sl)])
                            S.dma("sp", lambda e, sl=sl, cc=cc: e.dma_start(out=xc[:, sl, 3:515], in_=cqk[cc][:, 0:512]), writes=[("xc", sl)])
                        else:
                            S.dma("sp", lambda e, sl=sl, cc=cc, t0=t0: e.dma_start(out=xc[:, sl, :], in_=cqk[cc][:, t0 - 3:t0 + 512]), writes=[("xc", sl)])
                        S.op("dve", lambda e, sl=sl, cc=cc: e.tensor_scalar(out=acc[:, sl, :], in0=xc[:, sl, 3:515], scalar1=cw[:, cc, 3:4], scalar2=cb[:, cc:cc + 1], op0=ALU.mult, op1=ALU.add),
                             reads=[("xc", sl), "cw"], writes=[("acc", sl)])
                        for j in range(3):
                            S.op("dve", lambda e, sl=sl, cc=cc, j=j: e.scalar_tensor_tensor(out=acc[:, sl, :], in0=xc[:, sl, j:j + 512], scalar=cw[:, cc, j:j + 1], in1=acc[:, sl, :],
                                                                                            op0=ALU.mult, op1=ALU.add), reads=[("xc", sl), "cw", ("acc", sl)], writes=[("acc", sl)])
                        if cc < 4:
                            S.op("act", lambda e, sl=sl: e.activation(out=yb[:, sl, :], in_=acc[:, sl, :], func=AF.Silu), reads=[("acc", sl)], writes=[("yb", sl)])
                        else:
                            S.op("act", lambda e, sl=sl: e.activation(out=acc[:, sl, :], in_=acc[:, sl, :], func=AF.Silu), reads=[("acc", sl)], writes=[("acc", sl)])
                            S.op("pool", lambda e, sl=sl: e.tensor_scalar(out=yb[:, sl, :], in0=acc[:, sl, :], scalar1=float(128 ** -0.5), scalar2=None, op0=ALU.mult),
                                 reads=[("acc", sl)], writes=[("yb", sl)])
                        S.dma("sp", lambda e, sl=sl, cc=cc, t0=t0: e.dma_start(out=cqk2[cc][:, t0:t0 + 512], in_=yb[:, sl, :]), reads=[("yb", sl)])
                S.emit()
            G = T // 128
            P4 = 4 * G
            TS = min(T, 1024)
            NCS = TS // 64
            with ExitStack() as st:
                def sb_(name, shape, dt):
                    return st.enter_context(nc.sbuf_tensor(U(name), shape, dt))
                ip = sb_("ip", [P4, 128], F32)
                fp = sb_("fp", [P4, 128], F32)
                nbc = sb_("nbc", [P4, 128], F32)
                gg = sb_("gg", [P4, 128], F32)
                gmx = sb_("gmx", [P4, 128], F32)
                mt_ = sb_("mt_", [P4, 128], F32)
                tq = sb_("tq", [P4, 128], F32)
                rt_ = sb_("rt_", [P4, 128], F32)
                wtl = sb_("wtl", [P4, 128], F32)
                enm = sb_("enm", [P4, 128], F32)
                rm = sb_("rm", [P4, 2, 128], F32)
                bif = sb_("bif", [P4, 3], F32)
                ge = sb_("ge", [P4, 2], F32)
                nbe = sb_("nbe", [P4, 2], F32)
                mp = sb_("mp", [P4, 2], F32)
                ch = sb_("ch", [4, 8, NCH], F32)
                sel = sb_("sel", [4, 4, 128], F32)
                abc = sb_("abc", [128, 4, 2, NCH], F32)
                mneg = sb_("mneg", [64, 64], F32)
                gain = sb_("gain", [64, 512], F32)
                onec = sb_("onec", [64, 2], BF16)
                tokv = lambda d_: d_.rearrange("h (g t) -> (h g) t", t=128)
                chv = lambda d_: d_.rearrange("h (g c) -> (h g) c", c=2)
                S.dma("sp", [lambda e: e.dma_start(out=ip[:], in_=tokv(cif[0:4, :])), lambda e: e.dma_start(out=fp[:], in_=tokv(cif[4:8, :]))], writes=["ipfp"])
                S.dma("sp", [lambda e: e.dma_start(out=bif[:, 0:1], in_=dr["b_i"][l]), lambda e: e.dma_start(out=bif[:, 1:2], in_=dr["b_f"][l])], writes=["bif"])
                S.dma("sp", [lambda e: e.dma_start(out=rm[:, 0, :], in_=dr["c_rmask01"][0:P4, :]), lambda e: e.dma_start(out=rm[:, 1, :], in_=dr["c_rmaskneg"][0:P4, :])], writes=["rm"])
                S.dma("sp", lambda e: e.dma_start(out=sel[:], in_=dr["c_sel"]), writes=["sel"])
                S.dma("sp", lambda e: e.dma_start(out=mneg[:], in_=dr["c_maskneg"]), writes=["mneg"])
                S.dma("sp", lambda e: e.dma_start(out=gain[:], in_=dr["mlstm_norm"][l, 0].partition_broadcast(64)), writes=["gain"])
                S.op("pool", lambda e: e.memset(onec[:], 1.0), writes=["onec"])
                S.op("dve", lambda e: e.tensor_scalar(out=ip[:], in0=ip[:], scalar1=bif[:, 0:1], scalar2=None, op0=ALU.add), reads=["ipfp", "bif"], writes=["li"])
                S.op("dve", lambda e: e.tensor_scalar(out=bif[:, 2:3], in0=bif[:, 1:2], scalar1=-1.0, scalar2=None, op0=ALU.mult), reads=["bif"], writes=["bif2"])
                S.op("act", lambda e: e.activation(out=fp[:], in_=fp[:], func=AF.Exp, scale=-1.0, bias=bif[:, 2:3]), reads=["ipfp", "bif2"], writes=["sp"])
                S.op("act", lambda e: e.activation(out=fp[:], in_=fp[:], func=AF.Ln, bias=1.0), reads=["sp"], writes=["sp"])
                S.op("dve", lambda e: e.tensor_tensor_scan(out=nbc[:], data0=rm[:, 0, :], data1=fp[:], initial=0.0, op0=ALU.mult, op1=ALU.add), reads=["sp", "rm"], writes=["nb"])
                S.op("dve", lambda e: e.tensor_tensor(out=gg[:], in0=ip[:], in1=nbc[:], op=ALU.add), reads=["li", "nb"], writes=["g"])
                S.op("dve", lambda e: e.tensor_tensor_scan(out=gmx[:], data0=rm[:, 1, :], data1=gg[:], initial=0.0, op0=ALU.add, op1=ALU.max), reads=["g", "rm"], writes=["gmx"])
                ends = lambda t_: t_[:, :].rearrange("p (c t) -> p c t", t=64)[:, :, 63]
                S.op("dve", lambda e: e.tensor_copy(out=nbe[:], in_=ends(nbc)), reads=["nb"], writes=["nbe"])
                S.op("dve", lambda e: e.tensor_copy(out=ge[:], in_=ends(gmx)), reads=["gmx"], writes=["ge"])
                S.dma("sp", [lambda e: e.dma_start(out=chv(chd[0]), in_=nbe[:]), lambda e: e.dma_start(out=chv(chd[1]), in_=ge[:])], reads=["nbe", "ge"], writes=["chd01"])
                S.dma("sp", [lambda e: e.dma_start(out=ch[:, 0, :], in_=chd[0]), lambda e: e.dma_start(out=ch[:, 1, :], in_=chd[1])], reads=["chd01"], writes=["ch0", "ch1"])
                S.op("dve", lambda e: e.tensor_tensor(out=ch[:, 2, :], in0=ch[:, 1, :], in1=ch[:, 0, :], op=ALU.subtract), reads=["ch0", "ch1"], writes=["ch2"])
                S.op("dve", lambda e: e.tensor_scalar(out=ch[:, 3, :], in0=ch[:, 0, :], scalar1=-1.0, scalar2=None, op0=ALU.mult), reads=["ch0"], writes=["ch3"])
                S.op("dve", lambda e: e.tensor_tensor_scan(out=ch[:, 4, :], data0=ch[:, 3, :], data1=ch[:, 2, :], initial=0.0, op0=ALU.add, op1=ALU.max), reads=["ch2", "ch3"], writes=["ch4"])
                S.op("dve", lambda e: e.memset(ch[:, 5, 0:1], 0.0), writes=["ch5a"])
                if NCH > 1:
                    S.op("dve", lambda e: e.tensor_copy(out=ch[:, 5, 1:NCH], in_=ch[:, 4, 0:NCH - 1]), reads=["ch4"], writes=["ch5b"])
                S.dma("sp", lambda e: e.dma_start(out=chd[2], in_=ch[:, 5, :]), reads=["ch5a", "ch5b"], writes=["chd2"])
                S.dma("sp", lambda e: e.dma_start(out=mp[:], in_=chv(chd[2])), reads=["chd2"], writes=["mp"])
                S.op("dve", lambda e: e.tensor_tensor(out=ch[:, 6, :], in0=ch[:, 3, :], in1=ch[:, 5, :], op=ALU.add), reads=["ch3", "ch5a", "ch5b"], writes=["ch6"])
                S.op("dve", lambda e: e.tensor_tensor(out=ch[:, 6, :], in0=ch[:, 6, :], in1=ch[:, 4, :], op=ALU.subtract), reads=["ch6", "ch4"], writes=["ch6"])
                S.op("dve", lambda e: e.tensor_tensor(out=ch[:, 7, :], in0=ch[:, 2, :], in1=ch[:, 4, :], op=ALU.subtract), reads=["ch2", "ch4"], writes=["ch7"])
                S.op("act", lambda e: e.activation(out=ch[:, 6:8, :], in_=ch[:, 6:8, :], func=AF.Exp), reads=["ch6", "ch7"], writes=["ch67"])
                v3 = lambda t_: t_[:, :].rearrange("p (c t) -> p c t", t=64)
                bc = lambda t_: t_[:, :].unsqueeze(2).to_broadcast([P4, 2, 64])
                S.op("dve", lambda e: e.tensor_tensor(out=v3(tq), in0=bc(mp), in1=v3(nbc), op=ALU.subtract), reads=["mp", "nb"], writes=["tq"])
                S.op("dve", lambda e: e.tensor_tensor(out=mt_[:], in0=gmx[:], in1=nbc[:], op=ALU.subtract), reads=["gmx", "nb"], writes=["mt"])
                S.op("dve", lambda e: e.tensor_tensor(out=mt_[:], in0=mt_[:], in1=tq[:], op=ALU.max), reads=["mt", "tq"], writes=["mt"])
                S.op("dve", lambda e: e.tensor_tensor(out=tq[:], in0=tq[:], in1=mt_[:], op=ALU.subtract), reads=["tq", "mt"], writes=["tq"])
                S.op("act", lambda e: e.activation(out=tq[:], in_=tq[:], func=AF.Exp), reads=["tq"], writes=["tq"])
                S.op("dve", lambda e: e.scalar_tensor_tensor(out=rt_[:], in0=nbc[:], scalar=-1.0, in1=mt_[:], op0=ALU.mult, op1=ALU.subtract), reads=["nb", "mt"], writes=["rt"])
                S.op("act", lambda e: e.activation(out=enm[:], in_=mt_[:], func=AF.Exp, scale=-1.0), reads=["mt"], writes=["enm"])
                S.op("dve", lambda e: e.tensor_tensor(out=v3(wtl), in0=v3(gg), in1=bc(ge), op=ALU.subtract), reads=["g", "ge"], writes=["wtl"])
                S.op("act", lambda e: e.activation(out=wtl[:], in_=wtl[:], func=AF.Exp), reads=["wtl"], writes=["wtl"])
                S.dma("sp", [lambda e: e.dma_start(out=tokv(stkd[0:4, :]), in_=gg[:]), lambda e: e.dma_start(out=tokv(stkd[4:8, :]), in_=wtl[:]),
                             lambda e: e.dma_start(out=tokv(stkd[8:12, :]), in_=tq[:]), lambda e: e.dma_start(out=tokv(stkd[12:16, :]), in_=enm[:]),
                             lambda e: e.dma_start(out=tokv(rtd), in_=rt_[:])], reads=["g", "wtl", "tq", "enm", "rt"], writes=["stkd"])
                for h in range(4):
                    S.op("pe", lambda e, h=h: e.matmul(ps[0][:, 0:2 * NCH], sel[:, h, :], ch[:, 6:8, :], start=True, stop=True), reads=["ch67", "sel"], writes=[("ps", 0)])
                    S.op("act", lambda e, h=h: e.activation(out=abc[:, h], in_=ps[0][:, 0:2 * NCH].rearrange("p (a c) -> p a c", a=2), func=AF.Copy), reads=[("ps", 0)], writes=["abc"])
                S.emit()
                stk = sb_("stk", [16, TS], F32)
                rt = sb_("rt", [4, TS], F32)
                q_sb = sb_("q_sb", [128, 4, TS], BF16)
                k_sb = sb_("k_sb", [128, 4, TS], BF16)
                v_sb = sb_("v_sb", [64, NCS, 512], BF16)
                og = sb_("og", [64, NCS, 512], F32)
                cols = sb_("cols", [64, 16], F32)
                kw = sb_("kw", [64, 4, 128], BF16)
                ex = sb_("ex", [64, 4, 64], F32)
                AT = sb_("AT", [64, 4, 64], BF16)
                Cf = sb_("Cf", [128, 4, 128], F32)
                Cb = sb_("Cb", [128, 4, 128], BF16)
                nf = sb_("nf", [128, 4], F32)
                nbb = sb_("nbb", [128, 4], BF16)
                t2 = sb_("t2", [64, 4, 128], F32)
                dn = sb_("dn", [64, 8], F32)
                tmpC = sb_("tmpC", [128, 4, 128], F32)
                tmpn = sb_("tmpn", [128, 4], F32)
                sqt = sb_("sqt", [64, 4, 128], F32)
                ss = sb_("ss", [64, 4], F32)
                o_bf = sb_("o_bf", [64, 4, 128], BF16)
                stg = sb_("stg", [128, 2, 4, 512], BF16)
                S.op("dve", lambda e: e.memset(Cf[:], 0.0), writes=["Cf"])
                S.op("pool", lambda e: e.memset(Cb[:], 0.0), writes=["Cb"])
                S.op("dve", lambda e: e.memset(nf[:], 0.0), writes=["nf"])
                S.op("pool", lambda e: e.memset(nbb[:], 0.0), writes=["nbb"])
                for ts in range(T // TS):
                    t0s = ts * TS
                    S.dma("sp", [lambda e, h=h, t0s=t0s: e.dma_start(out=q_sb[:, h, :], in_=cqk2[h][:, t0s:t0s + TS]) for h in range(4)] +
                          [lambda e, h=h, t0s=t0s: e.dma_start(out=k_sb[:, h, :], in_=cqk2[4 + h][:, t0s:t0s + TS]) for h in range(4)], writes=["qk"])
                    S.dma("sp", lambda e, t0s=t0s: e.dma_start(out=v_sb[:], in_=cv[t0s:t0s + TS, :].rearrange("(c p) n -> p c n", p=64)), writes=["v"])
                    S.dma("sp", lambda e, t0s=t0s: e.dma_start(out=og[:], in_=co[t0s:t0s + TS, :].rearrange("(c p) n -> p c n", p=64)), writes=["og"])
                    S.dma("sp", [lambda e, t0s=t0s: e.dma_start(out=stk[:], in_=stkd[:, t0s:t0s + TS]), lambda e, t0s=t0s: e.dma_start(out=rt[:], in_=rtd[:, t0s:t0s + TS])], writes=["stk", "rt"])
                    for c in range(NCS):
                        t0 = c * 64
                        tg = t0s + t0
                        cg = tg // 64
                        S.op("pe", lambda e, t0=t0: e.transpose(ps[0][0:64, 0:16], stk[:, t0:t0 + 64], ident_f[0:16, 0:16]),
                             reads=["stk", "ident_f"], writes=[("ps", 0)])
                        S.op("act", lambda e: e.activation(out=cols[:], in_=ps[0][0:64, 0:16], func=AF.Copy), reads=[("ps", 0)], writes=["cols"])
                        kb_ = ps[1][:, 0:256].bitcast(BF16)
                        for h in range(4):
                            S.op("pe", lambda e, h=h, t0=t0: e.transpose(kb_[0:64, 128 * h:128 * h + 128], k_sb[:, h, t0:t0 + 64], ident_b[:]), reads=["qk", "ident_b"], writes=[("ps", 1)])
                        S.op("dve", lambda e: e.tensor_tensor(out=kw[:], in0=kb_[0:64, :].rearrange("p (h d) -> p h d", h=4), in1=cols[:, 4:8].unsqueeze(2).to_broadcast([64, 4, 128]), op=ALU.mult),
                             reads=[("ps", 1), "cols"], writes=["kw"])
                        for h in range(4):
                            S.op("pe", lambda e, h=h, t0=t0: e.matmul(ps[2][0:64, 64 * h:64 * h + 64], k_sb[:, h, t0:t0 + 64], q_sb[:, h, t0:t0 + 64], start=True, stop=True),
                                 reads=["qk"], writes=[("ps", 2)])
                        for h in range(4):
                            S.op("pe", lambda e, h=h, t0=t0: e.matmul(ps[3][0:64, 64 * h:64 * h + 64], sel[:, h, 0:64], rt[:, t0:t0 + 64], start=True, stop=False),
                                 reads=["sel", "rt"], writes=[("ps", 3)])
                            S.op("pe", lambda e, h=h: e.matmul(ps[3][0:64, 64 * h:64 * h + 64], ident_f[0:64, 0:64], mneg[:], start=False, stop=True),
                                 reads=["ident_f", "mneg"], writes=[("ps", 3)])
                        for h in range(4):
                            S.op("act", lambda e, h=h: e.activation(out=ex[:, h, :], in_=ps[3][0:64, 64 * h:64 * h + 64], func=AF.Exp, bias=cols[:, h:h + 1]),
                                 reads=[("ps", 3), "cols"], writes=[("ex", h)])
                        S.op("dve", lambda e: e.tensor_tensor(out=AT[:], in0=ex[:], in1=ps[2][0:64, 0:256].rearrange("p (h t) -> p h t", h=4), op=ALU.mult),
                             reads=[("ex", 0), ("ex", 1), ("ex", 2), ("ex", 3), ("ps", 2)], writes=["AT"])
                        for h in range(4):
                            S.op("pe", lambda e, h=h, c=c: e.matmul(ps[4][0:64, 128 * h:128 * h + 128], AT[:, h, :], v_sb[:, c, 128 * h:128 * h + 128], start=True, stop=True),
                                 reads=["AT", "v"], writes=[("ps", 4)])
                            S.op("pe", lambda e, h=h: e.matmul(ps[7][0:64, 16 + h:17 + h], AT[:, h, :], onec[:, 0:1], start=True, stop=True), reads=["AT", "onec"], writes=[("ps7", "a")])
                        for h in range(4):
                            S.op("pe", lambda e, h=h, t0=t0: e.matmul(ps[5][0:64, 128 * h:128 * h + 128], q_sb[:, h, t0:t0 + 64], Cb[:, h, :], start=True, stop=True),
                                 reads=["qk", "Cb"], writes=[("ps", 5)])
                            S.op("pe", lambda e, h=h, t0=t0: e.matmul(ps[7][0:64, 20 + h:21 + h], q_sb[:, h, t0:t0 + 64], nbb[:, h:h + 1], start=True, stop=True),
                                 reads=["qk", "nbb"], writes=[("ps7", "b")])
                        S.op("dve", lambda e: e.tensor_tensor(out=t2[:], in0=ps[5][0:64, :].rearrange("p (h d) -> p h d", h=4), in1=cols[:, 8:12].unsqueeze(2).to_broadcast([64, 4, 128]), op=ALU.mult),
                             reads=[("ps", 5), "cols"], writes=["t2"])
                        S.op("dve", lambda e: e.tensor_tensor(out=t2[:], in0=t2[:], in1=ps[4][0:64, :].rearrange("p (h d) -> p h d", h=4), op=ALU.add), reads=["t2", ("ps", 4)], writes=["t2"])
                        S.op("dve", lambda e: e.tensor_tensor(out=dn[:, 0:4], in0=ps[7][0:64, 20:24], in1=cols[:, 8:12], op=ALU.mult), reads=[("ps7", "b"), "cols"], writes=["dn"])
                        S.op("dve", lambda e: e.tensor_tensor(out=dn[:, 0:4], in0=dn[:, 0:4], in1=ps[7][0:64, 16:20], op=ALU.add), reads=["dn", ("ps7", "a")], writes=["dn"])
                        S.op("act", lambda e: e.activation(out=dn[:, 0:4], in_=dn[:, 0:4], func=AF.Abs), reads=["dn"], writes=["dn"])
                        S.op("dve", lambda e: e.tensor_tensor(out=dn[:, 0:4], in0=dn[:, 0:4], in1=cols[:, 12:16], op=ALU.max), reads=["dn", "cols"], writes=["dn"])
                        S.op("dve", lambda e: e.reciprocal(out=dn[:, 0:4], in_=dn[:, 0:4]), reads=["dn"], writes=["dn"])
                        S.op("dve", lambda e: e.tensor_tensor(out=t2[:], in0=t2[:], in1=dn[:, 0:4].unsqueeze(2).to_broadcast([64, 4, 128]), op=ALU.mult), reads=["t2", "dn"], writes=["t2"])
                        S.op("dve", lambda e, c=c: e.tensor_tensor(out=t2[:], in0=t2[:], in1=og[:, c, :].rearrange("p (h d) -> p h d", h=4), op=ALU.mult), reads=["t2", "og"], writes=["t2"])
                        head_norm(t2[:], "t2", 64, gain[:, :].rearrange("p (h d) -> p h d", h=4), "gain", o_bf[:], "o_bf", sqt[:], ss[:], "C")
                        cc = cg % 8
                        sg = (tg // 512) % 2
                        tb_ = ps[6][:, 0:128].bitcast(BF16)
                        for h in range(4):
                            S.op("pe", lambda e, h=h: e.transpose(tb_[:, 64 * h:64 * h + 64], o_bf[:, h, :], ident_b[0:64, 0:64]), reads=["o_bf", "ident_b"], writes=[("ps", 6)])
                        S.op("act", lambda e, sg=sg, cc=cc: e.activation(out=stg[:, sg, :, 64 * cc:64 * cc + 64], in_=tb_.rearrange("p (h t) -> p h t", h=4), func=AF.Copy),
                             reads=[("ps", 6)], writes=[("stg", sg)])
                        if cc == 7:
                            T0 = (tg // 512) * 512
                            S.dma("sp", lambda e, sg=sg, T0=T0: e.dma_start(out=brT[:, 8:12, T0:T0 + 512], in_=stg[:, sg]), reads=[("stg", sg)])
                        for h in range(4):
                            S.op("pe", lambda e, h=h, c=c: e.matmul(ps[1][:, 128 * h:128 * h + 128], kw[:, h, :], v_sb[:, c, 128 * h:128 * h + 128], start=True, stop=True),
                                 reads=["kw", "v"], writes=[("ps", 1)])
                            S.op("pe", lambda e, h=h: e.matmul(ps[7][:, 24 + h:25 + h], kw[:, h, :], onec[:, 0:1], start=True, stop=True), reads=["kw", "onec"], writes=[("ps7", "c")])
                        a_bc = lambda cg=cg: abc[:, :, 0, cg:cg + 1].to_broadcast([128, 4, 128])
                        b_bc = lambda cg=cg: abc[:, :, 1, cg:cg + 1].to_broadcast([128, 4, 128])
                        S.op("dve", lambda e, cg=cg: e.tensor_tensor(out=tmpC[:], in0=ps[1][:, :].rearrange("p (h d) -> p h d", h=4), in1=abc[:, :, 1, cg:cg + 1].to_broadcast([128, 4, 128]), op=ALU.mult),
                             reads=[("ps", 1), "abc"], writes=["tmpC"])
                        S.op("pool", lambda e, cg=cg: e.tensor_tensor(out=Cf[:], in0=Cf[:], in1=abc[:, :, 0, cg:cg + 1].to_broadcast([128, 4, 128]), op=ALU.mult), reads=["Cf", "abc"], writes=["Cf"])
                        S.op("pool", lambda e: e.tensor_tensor(out=Cf[:], in0=Cf[:], in1=tmpC[:], op=ALU.add), reads=["Cf", "tmpC"], writes=["Cf"])
                        S.op("act", lambda e: e.activation(out=Cb[:], in_=Cf[:], func=AF.Copy), reads=["Cf"], writes=["Cb"])
                        S.op("dve", lambda e, cg=cg: e.tensor_tensor(out=tmpn[:], in0=ps[7][:, 24:28], in1=abc[:, :, 1, cg], op=ALU.mult), reads=[("ps7", "c"), "abc"], writes=["tmpn"])
                        S.op("dve", lambda e, cg=cg: e.tensor_tensor(out=nf[:], in0=nf[:], in1=abc[:, :, 0, cg], op=ALU.mult), reads=["nf", "abc"], writes=["nf"])
                        S.op("dve", lambda e: e.tensor_tensor(out=nf[:], in0=nf[:], in1=tmpn[:], op=ALU.add), reads=["nf", "tmpn"], writes=["nf"])
                        S.op("dve", lambda e: e.tensor_copy(out=nbb[:], in_=nf[:]), reads=["nf"], writes=["nbb"])
                S.emit()

        stages = dbg if isinstance(dbg, dict) else {}
        upto = stages.get("upto", "all")
        phase_in()
        for l in range(L):
            phase_inproj(l)
            if upto == "inproj":
                break
            phase_gates(l)
            if "skipA" not in stages:
                mixer_A(l)
            if "skipB" not in stages:
                mixer_B(l)
            if "skipC" not in stages:
                mixer_C(l)
            if "skipD" not in stages:
                mixer_D(l)
            if upto == "mix":
                break
            phase_merge(l)
            if upto == "merge":
                break
            phase_outproj(l)
            if upto == "outproj":
                break
            phase_norm(dr["g_mix_post"][l], dr["g_ffn_pre"][l], False)
            if upto == "norm1":
                break
            phase_ffn_up(l)
            if upto == "up":
                break
            phase_ffn_down(l)
            if upto == "down":
                break
            last = (l == L - 1)
            phase_norm(dr["g_ffn_post"][l], None if last else dr["g_mix_pre"][l + 1], last)
    return nc


_CACHE = {}


def kernel(**inputs):
    T, L, NCORE = 4096, 4, 8
    x = np.asarray(inputs["x"], dtype=np.float32)
    if "nc" not in _CACHE:
        _CACHE["nc"] = build(T, L)
    nc = _CACHE["nc"]
    params = prep_params(inputs, L, T)
    cs = make_consts(T)
    shared = {k: np.ascontiguousarray(np.asarray(inputs[k], dtype=np.float32)) for k in BIGW(L)}
    shared.update(params)
    shared.update({"c_" + k: np.ascontiguousarray(v) for k, v in cs.items()})
    in_maps = []
    for b in range(NCORE):
        m = dict(shared)
        m["x"] = np.ascontiguousarray(x[b])
        in_maps.append(m)
    res = run_bass_kernel_spmd(nc, in_maps, core_ids=list(range(NCORE)))
    return np.stack([np.asarray(r["out"], dtype=np.float32) for r in res.results], axis=0)
```
